# Optimizing a Trainium2 kernel written in Bass

```python
import math
import jax, jax.numpy as jnp
from jax import lax
import numpy as np

D_MODEL = 1024
BATCH = 16
SEQ = 2048
DEPTH = 4

GRID_W = 64
HEAD_DIM = 64
POOL_WIDTH = D_MODEL // 4
POOL_WINDOWS = (2, 4, 8, 16)
POOL_GROUP = POOL_WIDTH // len(POOL_WINDOWS)
RWKV_WIDTH = D_MODEL // 4
RWKV_HEADS = RWKV_WIDTH // HEAD_DIM
DECAY_LORA = 32
AAA_LORA = 32
GATE_LORA = 64
GN_EPS = 64e-5
RWKV_IN = 3 * RWKV_WIDTH + 2 * DECAY_LORA + 2 * AAA_LORA + GATE_LORA
ATTN_WIDTH = D_MODEL // 2
ATTN_HEADS = ATTN_WIDTH // HEAD_DIM
ATTN_KV_HEADS = ATTN_HEADS // 4
ATTN_KV_WIDTH = ATTN_KV_HEADS * HEAD_DIM
Q_BLOCK = 128
ROPE_THETA = 10000.0
QK_EPS = 1e-6
IN_WIDTH = POOL_WIDTH + RWKV_IN + ATTN_WIDTH + 2 * ATTN_KV_WIDTH
N_GROUPS = 4
EXPERTS_PER_GROUP = 8
N_EXPERTS = N_GROUPS * EXPERTS_PER_GROUP
TOP_K = 2
EXPERT_HIDDEN = D_MODEL // 2
MOE_BLOCK = 256
DEEPNORM_ALPHA = float((2 * DEPTH) ** 0.25)
DEEPNORM_BETA = float((8 * DEPTH) ** -0.25)
LN_EPS = 1e-5

kernel_name = "hybrid_pool_rwkv7_axialgqa_hiermoe_encoder"


def layer_norm(x, g, b):
    xf = x.astype(jnp.float32)
    mu = jnp.mean(xf, -1, keepdims=True)
    var = jnp.mean(jnp.square(xf - mu), -1, keepdims=True)
    return ((xf - mu) * lax.rsqrt(var + LN_EPS) * g + b).astype(x.dtype)


def rms_norm(x, g):
    xf = x.astype(jnp.float32)
    return (xf * lax.rsqrt(jnp.mean(xf * xf, -1, keepdims=True) + QK_EPS) * g).astype(x.dtype)


def axial_rope_tables(seq_len):
    rows = seq_len // GRID_W
    row_id = jnp.repeat(jnp.arange(rows), GRID_W).astype(jnp.float32)
    col_id = jnp.tile(jnp.arange(GRID_W), rows).astype(jnp.float32)
    half = HEAD_DIM // 2
    inv_freq = ROPE_THETA ** (-jnp.arange(0, half, 2, dtype=jnp.float32) / half)
    ang_r = row_id[:, None] * inv_freq
    ang_c = col_id[:, None] * inv_freq
    ang = jnp.concatenate([ang_r, ang_r, ang_c, ang_c], -1)
    return jnp.cos(ang), jnp.sin(ang)


def apply_axial_rope(x, cos, sin):
    half = HEAD_DIM // 2
    quarter = half // 2
    def rot(u):
        return jnp.concatenate([-u[..., quarter:], u[..., :quarter]], -1)
    xr = jnp.concatenate([rot(x[..., :half]), rot(x[..., half:])], -1)
    c = cos[None, :, None, :].astype(x.dtype)
    s = sin[None, :, None, :].astype(x.dtype)
    return x * c + xr * s


def multiscale_pool(u, pool_w, pool_scale):
    S = u.shape[1]
    uf = u.astype(jnp.float32)
    cs = jnp.pad(jnp.cumsum(uf, axis=1), ((0, 0), (1, 0), (0, 0)))
    t = jnp.arange(S)
    outs = []
    for gi, w in enumerate(POOL_WINDOWS):
        sl = slice(gi * POOL_GROUP, (gi + 1) * POOL_GROUP)
        lo = jnp.clip(t - w // 2, 0, S)
        hi = jnp.clip(t + w - w // 2, 0, S)
        c = cs[:, :, sl]
        mean = (c[:, hi] - c[:, lo]) / (hi - lo).astype(jnp.float32)[None, :, None]
        d = (mean - uf[:, :, sl]).astype(u.dtype)
        outs.append(d @ pool_w[gi])
    return jnp.concatenate(outs, -1) * pool_scale


def centred_token_shift(f, mu_prev, mu_next):
    prev = jnp.pad(f[:, :-1], ((0, 0), (1, 0), (0, 0)))
    nxt = jnp.pad(f[:, 1:], ((0, 0), (0, 1), (0, 0)))
    return f + mu_prev * (prev - f) + mu_next * (nxt - f)


def wkv7_scan(r, w, k, v, kk, a, reverse):
    B, S, H, N = r.shape
    def step(state, inp):
        r_t, w_t, k_t, v_t, kk_t, a_t = inp
        sk = jnp.einsum('bhij,bhj->bhi', state, kk_t)
        state = (state * w_t[:, :, None, :]
                 - sk[..., None] * (kk_t * a_t)[:, :, None, :]
                 + v_t[..., None] * k_t[:, :, None, :])
        return state, jnp.einsum('bhij,bhj->bhi', state, r_t)
    xs = tuple(jnp.moveaxis(z, 1, 0) for z in (r, w, k, v, kk, a))
    s0 = jnp.zeros((B, H, N, N), jnp.float32)
    _, ys = lax.scan(step, s0, xs, reverse=reverse)
    return jnp.moveaxis(ys, 0, 1)


def rwkv7_bidir(f, w0, w_up, a0, a_up, g_up, k_k, k_a, r_k, gn_g, gn_b):
    B, S, _ = f.shape
    W = RWKV_WIDTH
    r, k, v = f[..., :W], f[..., W:2 * W], f[..., 2 * W:3 * W]
    o = 3 * W
    wd = f[..., o:o + 2 * DECAY_LORA].reshape(B, S, 2, DECAY_LORA)
    o += 2 * DECAY_LORA
    ad = f[..., o:o + 2 * AAA_LORA].reshape(B, S, 2, AAA_LORA)
    o += 2 * AAA_LORA
    gd = f[..., o:o + GATE_LORA]

    def heads(z):
        return z.reshape(B, S, RWKV_HEADS, HEAD_DIM).astype(jnp.float32)

    d = (w0 + jnp.einsum('bsdl,dlc->bsdc', jnp.tanh(wd), w_up)).astype(jnp.float32)
    decay = jnp.exp(-math.exp(-0.5) * jax.nn.sigmoid(d))
    a = jax.nn.sigmoid((a0 + jnp.einsum('bsdl,dlc->bsdc', ad, a_up)).astype(jnp.float32))
    g = jax.nn.sigmoid(gd) @ g_up
    kf = k.astype(jnp.float32)
    kk = heads(kf * k_k)
    kk = kk / jnp.maximum(jnp.sqrt(jnp.sum(kk * kk, -1, keepdims=True)), 1e-12)
    rh, vh = heads(r), heads(v)
    wkv = jnp.zeros_like(rh)
    bonus = jnp.zeros_like(rh)
    for di, rev in enumerate((False, True)):
        a_d = a[:, :, di]
        kh = heads(kf * (1.0 + (a_d - 1.0) * k_a))
        wkv = wkv + wkv7_scan(rh, heads(decay[:, :, di]), kh, vh, kk, heads(a_d), rev)
        bonus = bonus + jnp.sum(rh * kh * r_k, -1, keepdims=True) * vh
    mu = jnp.mean(wkv, -1, keepdims=True)
    var = jnp.mean(jnp.square(wkv - mu), -1, keepdims=True)
    y = ((wkv - mu) * lax.rsqrt(var + GN_EPS)).reshape(B, S, W) * gn_g + gn_b
    y = y + bonus.reshape(B, S, W)
    return (y * g).astype(f.dtype)


def gqa_axial(u, q_norm, k_norm, cos, sin):
    B, S, _ = u.shape
    q = u[..., :ATTN_WIDTH].reshape(B, S, ATTN_HEADS, HEAD_DIM)
    k = u[..., ATTN_WIDTH:ATTN_WIDTH + ATTN_KV_WIDTH].reshape(B, S, ATTN_KV_HEADS, HEAD_DIM)
    v = u[..., ATTN_WIDTH + ATTN_KV_WIDTH:].reshape(B, S, ATTN_KV_HEADS, HEAD_DIM)
    q = apply_axial_rope(rms_norm(q, q_norm), cos, sin)
    k = apply_axial_rope(rms_norm(k, k_norm), cos, sin)
    G = ATTN_HEADS // ATTN_KV_HEADS
    nb = S // Q_BLOCK
    qb = q.reshape(B, nb, Q_BLOCK, ATTN_KV_HEADS, G, HEAD_DIM).transpose(1, 0, 3, 4, 2, 5)
    scale = HEAD_DIM ** -0.5

    def block(q_blk):
        s = jnp.einsum('bkgqd,bskd->bkgqs', q_blk, k, preferred_element_type=jnp.float32) * scale
        p = jax.nn.softmax(s, axis=-1).astype(v.dtype)
        return jnp.einsum('bkgqs,bskd->bkgqd', p, v)

    o = lax.map(block, qb)
    return o.transpose(1, 0, 4, 2, 3, 5).reshape(B, S, ATTN_WIDTH)


def hier_moe(x, wg, bg, we, be, w_gate, w_up, w_down):
    B, S, D = x.shape
    N = B * S
    xt = x.reshape(N, D)
    glog = (xt @ wg).astype(jnp.float32) + bg
    grp = jnp.argmax(glog, -1)
    g_w = jnp.take_along_axis(jax.nn.softmax(glog, -1), grp[:, None], 1)
    elog = ((xt @ we).astype(jnp.float32) + be).reshape(N, N_GROUPS, EXPERTS_PER_GROUP)
    elog = jnp.take_along_axis(elog, grp[:, None, None], 1)[:, 0]
    top_v, top_i = lax.top_k(elog, TOP_K)
    gates = jax.nn.softmax(top_v, -1) * g_w
    eid = grp[:, None] * EXPERTS_PER_GROUP + top_i
    M = N * TOP_K
    e_flat = eid.reshape(M)
    tok = jnp.repeat(jnp.arange(N, dtype=jnp.int32), TOP_K)
    gate_flat = gates.reshape(M)
    order = jnp.argsort(e_flat)
    e_sorted = e_flat[order]
    counts = jnp.bincount(e_flat, length=N_EXPERTS)
    padded = (counts + MOE_BLOCK - 1) // MOE_BLOCK * MOE_BLOCK
    start = jnp.cumsum(counts) - counts
    ends_p = jnp.cumsum(padded)
    pstart = ends_p - padded
    dest = pstart[e_sorted] + jnp.arange(M) - start[e_sorted]
    n_blocks = -(-(M + N_EXPERTS * (MOE_BLOCK - 1)) // MOE_BLOCK)
    P = n_blocks * MOE_BLOCK
    row_tok = jnp.full((P,), N, jnp.int32).at[dest].set(tok[order])
    row_gate = jnp.zeros((P,), jnp.float32).at[dest].set(gate_flat[order])
    block_exp = jnp.minimum(
        jnp.searchsorted(ends_p, jnp.arange(n_blocks) * MOE_BLOCK, side='right'), N_EXPERTS - 1)
    x_pad = jnp.concatenate([xt, jnp.zeros((1, D), xt.dtype)], 0)
    xs = x_pad[row_tok].reshape(n_blocks, MOE_BLOCK, D)

    def expert_block(args):
        xb, e = args
        h = jax.nn.silu(xb @ w_gate[e]) * (xb @ w_up[e])
        return h @ w_down[e]

    ys = lax.map(expert_block, (xs, block_exp)).reshape(P, D)
    ys = ys * row_gate[:, None].astype(ys.dtype)
    out = jnp.zeros((N + 1, D), ys.dtype).at[row_tok].add(ys)[:N]
    return out.reshape(B, S, D)


def setup_inputs(seed: int = 0) -> dict:
    key = jax.random.key(seed)
    ks = iter(jax.random.split(key, 40))
    L, D = DEPTH, D_MODEL

    def nrm(shape, scale):
        return jax.random.normal(next(ks), shape, jnp.float32) * scale

    def unif(shape, lo, hi):
        return jax.random.uniform(next(ks), shape, jnp.float32, minval=lo, maxval=hi)

    return {
        "x": nrm((BATCH, SEQ, D), 1.0),
        "w_in": nrm((L, D, IN_WIDTH), D ** -0.5),
        "mu_prev": unif((L, RWKV_IN), 0.0, 0.5),
        "mu_next": unif((L, RWKV_IN), 0.0, 0.5),
        "pool_w": nrm((L, len(POOL_WINDOWS), POOL_GROUP, POOL_GROUP), POOL_GROUP ** -0.5),
        "pool_scale": 1.0 + nrm((L, POOL_WIDTH), 0.1),
        "rw_w0": nrm((L, 2, RWKV_WIDTH), 1.0) - 0.5,
        "rw_w_up": nrm((L, 2, DECAY_LORA, RWKV_WIDTH), 0.1),
        "rw_a0": nrm((L, 2, RWKV_WIDTH), 0.5),
        "rw_a_up": nrm((L, 2, AAA_LORA, RWKV_WIDTH), AAA_LORA ** -0.5),
        "rw_g_up": nrm((L, GATE_LORA, RWKV_WIDTH), GATE_LORA ** -0.5),
        "rw_k_k": 0.85 + nrm((L, RWKV_WIDTH), 0.05),
        "rw_k_a": 1.0 + nrm((L, RWKV_WIDTH), 0.05),
        "rw_r_k": nrm((L, RWKV_HEADS, HEAD_DIM), 0.1),
        "rw_gn_g": 1.0 + nrm((L, RWKV_WIDTH), 0.05),
        "rw_gn_b": nrm((L, RWKV_WIDTH), 0.02),
        "q_norm": 1.0 + nrm((L, HEAD_DIM), 0.05),
        "k_norm": 1.0 + nrm((L, HEAD_DIM), 0.05),
        "w_o": nrm((L, D, D), D ** -0.5 * DEEPNORM_BETA),
        "ln1_g": 1.0 + nrm((L, D), 0.05),
        "ln1_b": nrm((L, D), 0.02),
        "router_group": nrm((L, D, N_GROUPS), D ** -0.5),
        "router_group_b": nrm((L, N_GROUPS), 0.01),
        "router_expert": nrm((L, D, N_EXPERTS), D ** -0.5),
        "router_expert_b": nrm((L, N_EXPERTS), 0.01),
        "exp_gate": nrm((L, N_EXPERTS, D, EXPERT_HIDDEN), D ** -0.5),
        "exp_up": nrm((L, N_EXPERTS, D, EXPERT_HIDDEN), D ** -0.5),
        "exp_down": nrm((L, N_EXPERTS, EXPERT_HIDDEN, D), EXPERT_HIDDEN ** -0.5 * DEEPNORM_BETA),
        "ln2_g": 1.0 + nrm((L, D), 0.05),
        "ln2_b": nrm((L, D), 0.02),
    }


def reference(x, w_in, mu_prev, mu_next, pool_w, pool_scale, rw_w0, rw_w_up, rw_a0, rw_a_up,
              rw_g_up, rw_k_k, rw_k_a, rw_r_k, rw_gn_g, rw_gn_b, q_norm, k_norm, w_o,
              ln1_g, ln1_b, router_group, router_group_b, router_expert, router_expert_b,
              exp_gate, exp_up, exp_down, ln2_g, ln2_b):
    S = x.shape[1]
    cos, sin = axial_rope_tables(S)
    a0_end = POOL_WIDTH
    b0_end = POOL_WIDTH + RWKV_IN
    for l in range(DEPTH):
        proj = x @ w_in[l]
        y_pool = multiscale_pool(proj[..., :a0_end], pool_w[l], pool_scale[l])
        rw_in = centred_token_shift(proj[..., a0_end:b0_end], mu_prev[l], mu_next[l])
        y_rwkv = rwkv7_bidir(rw_in, rw_w0[l], rw_w_up[l], rw_a0[l], rw_a_up[l], rw_g_up[l],
                             rw_k_k[l], rw_k_a[l], rw_r_k[l], rw_gn_g[l], rw_gn_b[l])
        y_attn = gqa_axial(proj[..., b0_end:], q_norm[l], k_norm[l], cos, sin)
        y = jnp.concatenate([y_pool, y_rwkv, y_attn], -1) @ w_o[l]
        x = layer_norm(DEEPNORM_ALPHA * x + y, ln1_g[l], ln1_b[l])
        m = hier_moe(x, router_group[l], router_group_b[l], router_expert[l], router_expert_b[l],
                     exp_gate[l], exp_up[l], exp_down[l])
        x = layer_norm(DEEPNORM_ALPHA * x + m, ln2_g[l], ln2_b[l])
    return x
```

```python
import math
from contextlib import ExitStack
import numpy as np
import concourse.bass as bass
import concourse.mybir as mybir
from concourse.bass_utils import run_bass_kernel_spmd

F32 = mybir.dt.float32
BF16 = mybir.dt.bfloat16
I32 = mybir.dt.int32
U32 = mybir.dt.uint32
AF = mybir.ActivationFunctionType
ALU = mybir.AluOpType
AX = mybir.AxisListType

NCORES = 8
D = 1024
T = 2048
NSEQ = 2
NT = NSEQ * T
DEPTH = 4
INW = 1984
NE = 32
EH = 512
CAP = 384
ALPHA = float((2 * DEPTH) ** 0.25)
LN_EPS = 1e-5
GN_EPS = 64e-5
QK_EPS = 1e-6
C0 = -math.exp(-0.5)
NPP = 36

SAME_ENGINE_SYNC = True
NDQ = 8


class Sch:
    def __init__(self, nc, ctx):
        self.nc = nc
        self.E = {'pe': nc.tensor, 'dve': nc.vector, 'act': nc.scalar,
                  'pool': nc.gpsimd, 'sp': nc.sync}
        self.csem = {}
        self.ccount = {}
        self.semobj = {}
        for e in ('pe', 'dve', 'act', 'pool'):
            self.csem[e] = ctx.enter_context(nc.semaphore('c_' + e))
            self.semobj[id(self.csem[e])] = self.csem[e]
            self.ccount[e] = 0
        self.dsem = {}
        self.dcount = {}
        for q in ('sp', 'pool', 'act'):
            self.dsem[q] = [ctx.enter_context(nc.semaphore('d_%s%d' % (q, i))) for i in range(NDQ)]
            for s in self.dsem[q]:
                self.semobj[id(s)] = s
            self.dcount[q] = 0
        self.seen = {e: {} for e in self.E}
        self.bufs = {}
        self.nwaits = 0
        self.nops = 0

    def _buf(self, k):
        b = self.bufs.get(k)
        if b is None:
            b = {'w': {}, 'r': {}}
            self.bufs[k] = b
        return b

    def _gather(self, reads, writes):
        deps = {}
        for k in reads:
            for s, v in self._buf(k)['w'].items():
                if deps.get(s, 0) < v:
                    deps[s] = v
        for k in writes:
            b = self._buf(k)
            for d in (b['w'], b['r']):
                for s, v in d.items():
                    if deps.get(s, 0) < v:
                        deps[s] = v
        return deps

    def _wait(self, eng, deps):
        own = id(self.csem[eng]) if eng in self.csem else None
        seen = self.seen[eng]
        for s, v in deps.items():
            if seen.get(s, 0) >= v:
                continue
            if s == own and (eng == 'pe' or not SAME_ENGINE_SYNC):
                continue
            self.E[eng].wait_ge(self.semobj[s], v)
            self.nwaits += 1
            seen[s] = v

    def _record(self, reads, writes, s, v):
        for k in reads:
            r = self._buf(k)['r']
            if r.get(s, 0) < v:
                r[s] = v
        for k in writes:
            b = self._buf(k)
            b['w'] = {s: v}
            b['r'] = {}

    @staticmethod
    def _norm(reads, writes):
        r2, w2 = [], []
        for k in reads:
            if isinstance(k, tuple) and k[0] == 'ps':
                w2.append(k[:2])
            else:
                r2.append(k)
        for k in writes:
            if isinstance(k, tuple) and k[0] == 'ps':
                w2.append(k[:2])
            else:
                w2.append(k)
        return r2, w2

    def op(self, eng, fn, reads=(), writes=()):
        reads, writes = self._norm(reads, writes)
        deps = self._gather(reads, writes)
        self._wait(eng, deps)
        ins = fn(self.E[eng])
        self.ccount[eng] += 1
        v = self.ccount[eng]
        sem = self.csem[eng]
        ins.then_inc(sem, 1)
        self._record(reads, writes, id(sem), v)
        self.nops += 1
        return ins

    def dma(self, q, out, in_, reads=(), writes=(), fn=None, **kw):
        n = self.dcount[q]
        sem = self.dsem[q][n % NDQ]
        s = id(sem)
        prev = 16 * (n // NDQ)
        deps = self._gather(reads, writes)
        if prev > 0 and deps.get(s, 0) < prev:
            deps[s] = prev
        self._wait(q, deps)
        if fn is not None:
            ins = fn(self.E[q])
        else:
            ins = self.E[q].dma_start(out=out, in_=in_, **kw)
        ins.then_inc(sem, 16)
        self.dcount[q] = n + 1
        self._record(reads, writes, s, prev + 16)
        self.nops += 1
        return ins

    def barrier(self):
        deps = {}
        for e, sem in self.csem.items():
            if self.ccount[e] > 0:
                deps[id(sem)] = self.ccount[e]
        for q, sems in self.dsem.items():
            n = self.dcount[q]
            for i, sem in enumerate(sems):
                cnt = (n - i + NDQ - 1) // NDQ
                if cnt > 0:
                    deps[id(sem)] = 16 * cnt
        for e in self.E:
            own = id(self.csem[e]) if e in self.csem else None
            d = {s: v for s, v in deps.items() if s != own}
            self._wait(e, d)
        self.bufs = {}


def _consts():
    c = {}
    c['identF'] = np.eye(128, dtype=np.float32)
    r = np.arange(128)
    same = (r[:, None] // 64) == (r[None, :] // 64)
    c['mSL'] = (same & (r[None, :] < r[:, None])).astype(np.float32)
    c['mSU'] = (same & (r[:, None] < r[None, :])).astype(np.float32)
    c['mLI'] = (same & (r[None, :] <= r[:, None])).astype(np.float32)
    c['mUI'] = (same & (r[:, None] <= r[None, :])).astype(np.float32)
    c['bones'] = same.astype(np.float32)
    c['ustrict'] = (r[:, None] < r[None, :]).astype(np.float32)
    c['ones'] = np.ones((128, 128), np.float32)
    t = np.arange(512)
    c['rmask'] = np.broadcast_to((t % 64 != 0).astype(np.float32), (128, 512)).copy()
    rows = T // 64
    row_id = np.repeat(np.arange(rows), 64).astype(np.float32)
    col_id = np.tile(np.arange(64), rows).astype(np.float32)
    half = 32
    inv_freq = (10000.0 ** (-np.arange(0, half, 2, dtype=np.float32) / half)).astype(np.float32)
    ang_r = row_id[:, None] * inv_freq
    ang_c = col_id[:, None] * inv_freq
    ang = np.concatenate([ang_r, ang_r, ang_c, ang_c], -1)
    c['cosT'] = np.cos(ang).astype(np.float32)
    sn = np.sin(ang).astype(np.float32)
    sgn = np.ones(64, np.float32)
    sgn[0:16] = -1.0
    sgn[32:48] = -1.0
    c['sinS'] = sn * sgn
    ic = np.zeros((2, 128, T), np.float32)
    tt = np.arange(T)
    for gi, w in enumerate((2, 4, 8, 16)):
        lo = np.clip(tt - w // 2, 0, T)
        hi = np.clip(tt + w - w // 2, 0, T)
        ic[gi // 2, (gi % 2) * 64:(gi % 2) * 64 + 64, :] = 1.0 / (hi - lo).astype(np.float32)
    c['icnt'] = ic
    c['slotb'] = np.broadcast_to((np.arange(NE) * CAP).astype(np.float32), (128, NE)).copy()
    return c


CONST_SHAPES = {k: v.shape for k, v in _consts().items()}

WNAMES = ['w_in', 'pool_w', 'rw_w_up', 'rw_a_up', 'rw_g_up', 'w_o', 'router_group', 'router_expert',
          'exp_gate', 'exp_up', 'exp_down', 'pp', 'rowp']
NROW = 2 * 64 + 4 * 1024 + 36


def pack_small(inp, l):
    pp = np.zeros((128, NPP), np.float32)
    mp = np.zeros(1024, np.float32); mp[:960] = inp['mu_prev'][l]
    mn = np.zeros(1024, np.float32); mn[:960] = inp['mu_next'][l]
    pp[:, 0:8] = mp.reshape(8, 128).T
    pp[:, 8:16] = mn.reshape(8, 128).T
    pp[:, 16:18] = inp['pool_scale'][l].reshape(2, 128).T
    pp[:, 18:22] = inp['rw_w0'][l].reshape(4, 128).T
    pp[:, 22:26] = inp['rw_a0'][l].reshape(4, 128).T
    pp[:, 26:28] = inp['rw_k_k'][l].reshape(2, 128).T
    pp[:, 28:30] = inp['rw_k_a'][l].reshape(2, 128).T
    pp[:, 30:32] = inp['rw_r_k'][l].reshape(2, 128).T
    pp[:, 32:34] = inp['rw_gn_g'][l].reshape(2, 128).T
    pp[:, 34:36] = inp['rw_gn_b'][l].reshape(2, 128).T
    rowp = np.concatenate([inp['q_norm'][l], inp['k_norm'][l], inp['ln1_g'][l], inp['ln1_b'][l],
                           inp['ln2_g'][l], inp['ln2_b'][l], inp['router_group_b'][l],
                           inp['router_expert_b'][l]]).astype(np.float32)
    return pp, rowp[None, :]


class Prog:
    def __init__(self, nl, dbg=None, stages=None):
        self.nl = nl
        self.dbg = dbg or {}
        self.stages = stages
        nc = self.nc = bass.Bass("TRN2", target_bir_lowering=False)
        self.ctx = ExitStack()
        ctx = self.ctx
        dt_in = lambda n, shp: nc.dram_tensor(n, list(shp), F32, kind="ExternalInput").ap()
        self.x_in = dt_in('x', [NT, D])
        self.W = {}
        self.W['w_in'] = dt_in('w_in', [nl, D, INW])
        self.W['pool_w'] = dt_in('pool_w', [nl, 4, 64, 64])
        self.W['rw_w_up'] = dt_in('rw_w_up', [nl, 2, 32, 256])
        self.W['rw_a_up'] = dt_in('rw_a_up', [nl, 2, 32, 256])
        self.W['rw_g_up'] = dt_in('rw_g_up', [nl, 64, 256])
        self.W['w_o'] = dt_in('w_o', [nl, D, D])
        self.W['router_group'] = dt_in('router_group', [nl, D, 4])
        self.W['router_expert'] = dt_in('router_expert', [nl, D, NE])
        self.W['exp_gate'] = dt_in('exp_gate', [nl, NE, D, EH])
        self.W['exp_up'] = dt_in('exp_up', [nl, NE, D, EH])
        self.W['exp_down'] = dt_in('exp_down', [nl, NE, EH, D])
        self.W['pp'] = dt_in('pp', [nl, 128, NPP])
        self.W['rowp'] = dt_in('rowp', [nl, 1, NROW])
        self.C = {k: dt_in('c_' + k, shp) for k, shp in CONST_SHAPES.items()}
        self.out = nc.dram_tensor('out', [NT, D], F32, kind="ExternalOutput").ap()
        self.dbg_out = {}
        for k, shp in self.dbg.items():
            self.dbg_out[k] = nc.dram_tensor('dbg_' + k, list(shp), F32, kind="ExternalOutput").ap()
        itn = lambda n, shp, dt: nc.dram_tensor(n, list(shp), dt, kind="Internal").ap()
        self.fT = itn('fT', [1024, T], F32)
        self.x1d = itn('x1d', [NT, D], F32)
        self.xres = itn('xres', [NT, D], F32)
        self.xs = itn('xs', [NE * CAP + 128, D], BF16)
        self.ys = itn('ys', [NE * CAP + 128, D], F32)
        self.S = Sch(nc, ctx)
        self._uid = 0

        def sb(n, shp, dt, _ctx=ctx):
            self._uid += 1
            return _ctx.enter_context(nc.sbuf_tensor('%s_u%d' % (n, self._uid), list(shp), dt))
        self.sb = sb
        self.PS = [ctx.enter_context(nc.psum_tensor('ps%d' % i, [128, 512], F32)) for i in range(8)]
        self.identF = sb('identF', [128, 128], F32)
        self.identB = sb('identB', [128, 128], BF16)
        self.masks = {k: sb(k, [128, 128], BF16) for k in ('mSL', 'mSU', 'mLI', 'mUI')}
        self.bonesF = sb('bonesF', [128, 128], F32)
        self.bonesB = sb('bonesB', [128, 128], BF16)
        self.ustrict = sb('ustrict', [128, 128], BF16)
        self.onesB = sb('onesB', [128, 128], BF16)
        self.slotb = sb('slotb', [128, NE], F32)
        self.pp = sb('pp', [128, NPP], F32)
        self.ppd = sb('ppd', [128, 8], F32)
        self.yT = sb('yT', [128, 8, T], BF16)
        self.ridx = [[sb('ridx%d_%d' % (g_, j_), [128, 1], I32) for j_ in range(2)] for g_ in range(32)]
        self.rgate = sb('rgate', [128, 32, 2], F32)
        self.runc = sb('runc', [128, NE], F32)
        S = self.S
        S.dma('sp', self.identF[:], self.C['identF'], writes=['identF'])
        S.dma('pool', self.identB[:], self.C['identF'], writes=['identB'])
        for k in self.masks:
            S.dma('pool', self.masks[k][:], self.C[k], writes=[k])
        S.dma('sp', self.bonesF[:], self.C['bones'], writes=['bonesF'])
        S.dma('pool', self.bonesB[:], self.C['bones'], writes=['bonesB'])
        S.dma('pool', self.ustrict[:], self.C['ustrict'], writes=['ustrict'])
        S.dma('pool', self.onesB[:], self.C['ones'], writes=['onesB'])
        S.dma('sp', self.slotb[:], self.C['slotb'], writes=['slotb'])
        with nc.sbuf_tensor('zinit', [128, D], F32) as zt:
            S.op('dve', lambda e: e.memset(zt[:], 0.0), writes=['zt'])
            S.dma('sp', self.ys[NE * CAP:NE * CAP + 128, :], zt[:], reads=['zt'], writes=['ys'])
            S.barrier()

    def ps(self, i):
        return self.PS[i]

    def psb(self, i):
        return self.PS[i][:].bitcast(BF16)

    def finish(self):
        S = self.S
        S.barrier()
        self.ctx.close()
        return self.nc

    def want(self, name):
        return self.stages is None or name in self.stages

    def layer_setup(self, l):
        nc, S = self.nc, self.S
        S.dma('sp', self.pp[:], self.W['pp'][l], writes=['pp'])
        self.csh = self.sb('csh%d' % l, [128, 8], F32)
        self.oka = self.sb('oka%d' % l, [128, 2], F32)
        S.op('dve', lambda e: e.tensor_tensor(out=self.csh[:], in0=self.pp[:, 0:8], in1=self.pp[:, 8:16], op=ALU.add),
             reads=['pp'], writes=['csh'])
        S.op('dve', lambda e: e.tensor_scalar(out=self.csh[:], in0=self.csh[:], scalar1=-1.0, scalar2=1.0,
                                              op0=ALU.mult, op1=ALU.add), reads=['csh'], writes=['csh'])
        S.op('dve', lambda e: e.memset(self.runc[:], 0.0), writes=['runc'])
        S.op('dve', lambda e: e.tensor_scalar(out=self.oka[:], in0=self.pp[:, 28:30], scalar1=-1.0, scalar2=1.0,
                                              op0=ALU.mult, op1=ALU.add), reads=['pp'], writes=['oka'])
        S.barrier()

    def stage_A(self, l, s, xsrc, QT, KT, Vaug):
        nc, S = self.nc, self.S
        PS = self.PS
        pp = self.pp
        rowp = self.W['rowp'][l]
        with ExitStack() as st:
            sb = lambda n, shp, dt: self.sb(n, shp, dt, st)
            xT = sb('xT', [128, 8, T], BF16)
            win = sb('win', [128, 8, INW], BF16)
            xt = [sb('xt%d' % i, [128, D], F32) for i in range(2)]
            rb = [sb('rb%d' % i, [128, T + 16], F32) for i in range(2)]
            tA = sb('tA', [128, T + 16], F32)
            tB = sb('tB', [128, T + 16], F32)
            dbf = sb('dbf', [128, T], BF16)
            icn = sb('icn', [128, T], F32)
            dm = icn
            pwbd = sb('pwbd', [128, 2, 128], BF16)
            gqk = sb('gqk', [128, 640], F32)
            cs = [sb('cs%d' % i, [128, 128], F32) for i in range(2)]
            qkv = [sb('qkv%d' % i, [128, 768], F32) for i in range(2)]
            wk1 = sb('wk1', [128, 640], F32)
            wk2 = sb('wk2', [128, 640], F32)
            ss = sb('ss', [128, 10], F32)
            qrb = [sb('qrb%d' % i, [128, 768], BF16) for i in range(2)]
            for k in range(8):
                S.dma('pool', win[:, k, :], self.W['w_in'][l, k * 128:(k + 1) * 128, :], writes=[('win', k)])
            S.op('dve', lambda e: e.memset(pwbd[:], 0.0), writes=['pwbd'])
            for gi in range(4):
                h = (gi % 2) * 64
                S.dma('pool', pwbd[h:h + 64, gi // 2, h:h + 64], self.W['pool_w'][l, gi], reads=[], writes=['pwbd'])
            for h in range(10):
                off = 0 if h < 8 else 64
                S.dma('sp', gqk[:, h * 64:(h + 1) * 64], rowp[:, off:off + 64].broadcast_to([128, 64]), writes=['gqk'])
            for i in range(2):
                S.op('dve', lambda e: e.memset(rb[i][:, 0:8], 0.0), writes=[('rb', i)])
                S.op('dve', lambda e: e.memset(rb[i][:, 8 + T:], 0.0), writes=[('rb', i)])
            S.op('pool', lambda e: e.memset(Vaug[:, :, :, 64:128], 1.0), writes=['Vaug'])
            for ti in range(16):
                xb = xt[ti % 2]
                S.dma('sp', xb[:], xsrc[s * T + ti * 128: s * T + (ti + 1) * 128, :], writes=[('xt', ti % 2)])
                for hf in range(2):
                    pi = (ti * 2 + hf) % 4
                    for c in range(4):
                        k = hf * 4 + c
                        S.op('pe', lambda e: e.transpose(PS[pi][:, c * 128:(c + 1) * 128], xb[:, k * 128:(k + 1) * 128],
                                                         self.identF[:]),
                             reads=[('xt', ti % 2), 'identF'], writes=[('ps', pi)])
                    src = PS[pi][:, :].rearrange("p (c t) -> p c t", c=4)
                    dst = xT[:, hf * 4:hf * 4 + 4, ti * 128:(ti + 1) * 128]
                    if hf == 0:
                        S.op('act', lambda e: e.copy(out=dst, in_=src), reads=[('ps', pi)], writes=[('xT', ti)])
                    else:
                        S.op('dve', lambda e: e.tensor_copy(out=dst, in_=src), reads=[('ps', pi)], writes=[('xT', ti)])
            xT_all = [('xT', ti) for ti in range(16)]
            win_all = [('win', k) for k in range(8)]
            for mt in range(10):
                msz = 128 if mt < 9 else 64
                r = rb[mt % 2]
                rk = ('rb', mt % 2)
                for tb in range(4):
                    pi = (mt * 4 + tb) % 4
                    for k in range(8):
                        S.op('pe', lambda e: e.matmul(PS[pi][0:msz, :], win[:, k, mt * 128:mt * 128 + msz],
                                                      xT[:, k, tb * 512:(tb + 1) * 512], start=(k == 0), stop=(k == 7)),
                             reads=xT_all + win_all, writes=[('ps', pi)])
                    dst = r[0:msz, 8 + tb * 512: 8 + (tb + 1) * 512]
                    if tb % 2 == 0:
                        S.op('act', lambda e: e.copy(out=dst, in_=PS[pi][0:msz, :]), reads=[('ps', pi)], writes=[rk])
                    else:
                        S.op('dve', lambda e: e.tensor_copy(out=dst, in_=PS[pi][0:msz, :]), reads=[('ps', pi)], writes=[rk])
                if mt >= 2:
                    m = mt - 2
                    f = tA if m % 2 == 0 else tB
                    fk = 'tA' if m % 2 == 0 else 'tB'
                    S.op('dve', lambda e: e.tensor_scalar(out=f[0:msz, 0:T], in0=r[0:msz, 8:8 + T], scalar1=self.csh[0:msz, m:m + 1],
                                                          scalar2=None, op0=ALU.mult), reads=[rk, 'csh'], writes=[fk])
                    S.op('dve', lambda e: e.scalar_tensor_tensor(out=f[0:msz, 0:T], in0=r[0:msz, 7:7 + T], scalar=pp[0:msz, m:m + 1],
                                                                 in1=f[0:msz, 0:T], op0=ALU.mult, op1=ALU.add),
                         reads=[rk, 'pp', fk], writes=[fk])
                    S.op('dve', lambda e: e.scalar_tensor_tensor(out=f[0:msz, 0:T], in0=r[0:msz, 9:9 + T], scalar=pp[0:msz, 8 + m:9 + m],
                                                                 in1=f[0:msz, 0:T], op0=ALU.mult, op1=ALU.add),
                         reads=[rk, 'pp', fk], writes=[fk])
                    S.dma('sp', self.fT[m * 128:m * 128 + msz, :], f[0:msz, 0:T], reads=[fk], writes=[('fT', m)])
                else:
                    ic = tB if mt == 0 else None
                    add = lambda o, a, b, rd, wr: S.op('dve', lambda e: e.tensor_tensor(out=o, in0=a, in1=b, op=ALU.add), reads=rd, writes=wr)
                    n2 = T + 15
                    add(tA[:, 0:n2], r[:, 0:n2], r[:, 1:n2 + 1], [rk], ['tA'])
                    n4 = T + 13
                    add(tB[:, 0:n4], tA[:, 0:n4], tA[:, 2:n4 + 2], ['tA'], ['tB'])
                    if mt == 0:
                        lo_src, lo_off, hi_src, hi_off, lok, hik = tA, 7, tB, 6, 'tA', 'tB'
                    else:
                        n8 = T + 9
                        add(tA[:, 0:n8], tB[:, 0:n8], tB[:, 4:n8 + 4], ['tB'], ['tA'])
                        n16 = T + 1
                        add(tB[:, 0:n16], tA[:, 0:n16], tA[:, 8:n16 + 8], ['tA'], ['tB'])
                        lo_src, lo_off, hi_src, hi_off, lok, hik = tA, 4, tB, 0, 'tA', 'tB'
                    S.dma('sp', icn[:], self.C['icnt'][mt], writes=['icn'])
                    S.op('dve', lambda e: e.tensor_tensor(out=dm[0:64, :], in0=lo_src[0:64, lo_off:lo_off + T], in1=icn[0:64, :], op=ALU.mult),
                         reads=[lok, 'icn'], writes=['icn'])
                    S.op('dve', lambda e: e.tensor_tensor(out=dm[64:128, :], in0=hi_src[64:128, hi_off:hi_off + T], in1=icn[64:128, :], op=ALU.mult),
                         reads=[hik, 'icn'], writes=['icn'])
                    S.op('dve', lambda e: e.tensor_tensor(out=dbf[:], in0=dm[:], in1=r[:, 8:8 + T], op=ALU.subtract),
                         reads=['icn', rk], writes=['dbf'])
                    for tb in range(4):
                        pi = 4 + tb % 2
                        S.op('pe', lambda e: e.matmul(PS[pi][:, :], pwbd[:, mt, :], dbf[:, tb * 512:(tb + 1) * 512], start=True, stop=True),
                             reads=['pwbd', 'dbf'], writes=[('ps', pi)])
                        S.op('act', lambda e: e.activation(out=self.yT[:, mt, tb * 512:(tb + 1) * 512], in_=PS[pi][:, :], func=AF.Identity,
                                                           scale=pp[:, 16 + mt:17 + mt]),
                             reads=[('ps', pi), 'pp'], writes=[('yT', mt)])
            for ti in range(16):
                b = ti % 2
                pq, pkv = 4 + b * 2, 5 + b * 2
                for k in range(8):
                    S.op('pe', lambda e: e.matmul(PS[pq][:, :], xT[:, k, ti * 128:(ti + 1) * 128], win[:, k, 1216:1728],
                                                  start=(k == 0), stop=(k == 7)), reads=xT_all + win_all, writes=[('ps', pq)])
                for k in range(8):
                    S.op('pe', lambda e: e.matmul(PS[pkv][:, 0:256], xT[:, k, ti * 128:(ti + 1) * 128], win[:, k, 1728:1984],
                                                  start=(k == 0), stop=(k == 7)), reads=xT_all + win_all, writes=[('ps', pkv)])
                qk = qkv[b]
                qkk = ('qkv', b)
                S.op('act', lambda e: e.copy(out=qk[:, 0:512], in_=PS[pq][:, :]), reads=[('ps', pq)], writes=[qkk])
                S.op('act', lambda e: e.copy(out=qk[:, 512:768], in_=PS[pkv][:, 0:256]), reads=[('ps', pkv)], writes=[qkk])
                c_t = cs[b]
                S.dma('sp', c_t[:, 0:64], self.C['cosT'][ti * 128:(ti + 1) * 128, :], writes=[('cs', b)])
                S.dma('sp', c_t[:, 64:128], self.C['sinS'][ti * 128:(ti + 1) * 128, :], writes=[('cs', b)])
                S.op('dve', lambda e: e.tensor_tensor(out=wk1[:], in0=qk[:, 0:640], in1=qk[:, 0:640], op=ALU.mult), reads=[qkk], writes=['wk1'])
                S.op('dve', lambda e: e.tensor_reduce(out=ss[:], in_=wk1[:].rearrange("p (h d) -> p h d", h=10), axis=AX.X, op=ALU.add),
                     reads=['wk1'], writes=['ss'])
                S.op('act', lambda e: e.activation(out=ss[:], in_=ss[:], func=AF.Sqrt, bias=QK_EPS, scale=1.0 / 64), reads=['ss'], writes=['ss'])
                S.op('dve', lambda e: e.reciprocal(out=ss[:], in_=ss[:]), reads=['ss'], writes=['ss'])
                S.op('dve', lambda e: e.tensor_tensor(out=wk1[:].rearrange("p (h d) -> p h d", h=10),
                                                      in0=qk[:, 0:640].rearrange("p (h d) -> p h d", h=10),
                                                      in1=ss[:, :].unsqueeze(2).broadcast_to([128, 10, 64]), op=ALU.mult),
                     reads=[qkk, 'ss'], writes=['wk1'])
                S.op('dve', lambda e: e.tensor_tensor(out=wk1[:], in0=wk1[:], in1=gqk[:], op=ALU.mult), reads=['wk1', 'gqk'], writes=['wk1'])
                v4 = lambda ap: ap.rearrange("p (h a q d) -> p h a q d", h=10, a=2, q=2)
                cos4 = c_t[:, 0:64].rearrange("p (a q d) -> p a q d", a=2, q=2)
                sin4 = c_t[:, 64:128].rearrange("p (a q d) -> p a q d", a=2, q=2)
                for a in range(2):
                    for q in range(2):
                        S.op('dve', lambda e: e.tensor_tensor(out=v4(wk2[:])[:, :, a, q, :], in0=v4(wk1[:])[:, :, a, 1 - q, :],
                                                              in1=sin4[:, a, q, :].unsqueeze(1).broadcast_to([128, 10, 16]), op=ALU.mult),
                             reads=['wk1', ('cs', b)], writes=['wk2'])
                S.op('dve', lambda e: e.tensor_tensor(out=wk1[:].rearrange("p (h d) -> p h d", h=10),
                                                      in0=wk1[:].rearrange("p (h d) -> p h d", h=10),
                                                      in1=c_t[:, 0:64].unsqueeze(1).broadcast_to([128, 10, 64]), op=ALU.mult),
                     reads=['wk1', ('cs', b)], writes=['wk1'])
                qr = qrb[b]
                qrk = ('qrb', b)
                S.op('dve', lambda e: e.tensor_tensor(out=qr[:, 0:512], in0=wk1[:, 0:512], in1=wk2[:, 0:512], op=ALU.add),
                     reads=['wk1', 'wk2'], writes=[qrk])
                for du in range(2):
                    S.op('dve', lambda e: e.tensor_tensor(out=qr[:, 512:768].rearrange("p (k u d) -> p k u d", k=2, u=2)[:, :, du, :],
                                                          in0=wk1[:, 512:640].rearrange("p (k d) -> p k d", k=2),
                                                          in1=wk2[:, 512:640].rearrange("p (k d) -> p k d", k=2), op=ALU.add),
                         reads=['wk1', 'wk2'], writes=[qrk])
                pt = 0 + b
                psb = self.psb(pt)
                for j in range(6):
                    S.op('pe', lambda e: e.transpose(psb[:, j * 128:(j + 1) * 128], qr[:, j * 128:(j + 1) * 128], self.identB[:]),
                         reads=[qrk, 'identB'], writes=[('ps', pt)])
                S.op('act', lambda e: e.copy(out=QT[:, :, ti * 128:(ti + 1) * 128], in_=psb[:, 0:512].rearrange("p (c t) -> p c t", c=4)),
                     reads=[('ps', pt)], writes=[('QT', ti)])
                S.op('act', lambda e: e.copy(out=KT[:, :, ti * 128:(ti + 1) * 128], in_=psb[:, 512:768].rearrange("p (c t) -> p c t", c=2)),
                     reads=[('ps', pt)], writes=[('KT', ti)])
                S.op('pool', lambda e: e.tensor_copy(out=Vaug[:, ti, :, 0:64], in_=qk[:, 640:768].rearrange("p (k d) -> p k d", k=2)),
                     reads=[qkk], writes=['Vaug'])
            S.barrier()

    def stage_D(self, QT, KT, Vaug):
        nc, S = self.nc, self.S
        PS = self.PS
        with ExitStack() as st:
            sb = lambda n, shp, dt: self.sb(n, shp, dt, st)
            NB = 4
            LOOK = 3
            pT = [sb('pT%d' % i, [128, 512], BF16) for i in range(NB)]
            rsum = [sb('rsum%d' % i, [64, 512], F32) for i in range(4)]
            allq = [('QT', ti) for ti in range(16)] + [('KT', ti) for ti in range(16)]
            seq = [(2 * pr + hh, qb, kc) for pr in range(4) for qb in range(4) for kc in range(16) for hh in range(2)]

            def emit_S(i):
                h, qb, kc = seq[i]
                kv, pr, hf = h // 4, h // 2, (h % 2) * 64
                pi = i % NB
                S.op('pe', lambda e: e.matmul(PS[pi][:, :], KT[hf:hf + 64, kv, kc * 128:(kc + 1) * 128],
                                              QT[hf:hf + 64, pr, qb * 512:(qb + 1) * 512], start=True, stop=True),
                     reads=allq, writes=[('ps', pi)])
                S.op('act', lambda e: e.activation(out=pT[pi][:], in_=PS[pi][:, :], func=AF.Exp, scale=0.125),
                     reads=[('ps', pi)], writes=[('pT', pi)])

            def emit_PV(i):
                h, qb, kc = seq[i]
                kv, pr, hf = h // 4, h // 2, (h % 2) * 64
                pi = i % NB
                blk = i // 32
                po = 4 + (blk % 2) * 2 + (h % 2)
                S.op('pe', lambda e: e.matmul(PS[po][:, :], Vaug[:, kc, kv, :], pT[pi][:], start=(kc == 0), stop=(kc == 15)),
                     reads=[('pT', pi), 'Vaug'], writes=[('ps', po)])
                if kc == 15:
                    ri = (blk % 2) * 2 + (h % 2)
                    rs = rsum[ri]
                    rsk = ('rsum', ri)
                    S.op('act', lambda e: e.copy(out=rs[:], in_=PS[po][64:128, :]), reads=[('ps', po)], writes=[rsk])
                    S.op('dve', lambda e: e.reciprocal(out=rs[:], in_=rs[:]), reads=[rsk], writes=[rsk])
                    S.op('dve', lambda e: e.tensor_tensor(out=self.yT[hf:hf + 64, 4 + pr, qb * 512:(qb + 1) * 512], in0=PS[po][0:64, :],
                                                          in1=rs[:], op=ALU.mult), reads=[('ps', po), rsk], writes=[('yT', 4 + pr, hf)])

            for i in range(len(seq) + LOOK):
                if i < len(seq):
                    emit_S(i)
                if i >= LOOK:
                    emit_PV(i - LOOK)
            S.barrier()

    def stage_C(self, l, s):
        nc, S = self.nc, self.S
        PS = self.PS
        pp = self.pp
        fT = self.fT
        with ExitStack() as st:
            sb = lambda n, shp, dt: self.sb(n, shp, dt, st)
            wup = sb('wup', [64, 256], BF16)
            aup = sb('aup', [64, 256], BF16)
            loa = sb('loa', [64, T], BF16)
            gup = sb('gup', [64, 256], BF16)
            lo = sb('lo', [64, T], BF16)
            sgd = sb('sgd', [64, T], BF16)
            rmask = sb('rmask', [128, 512], F32)
            S.dma('pool', wup[:], self.W['rw_w_up'][l].rearrange("d l c -> (d l) c"), writes=['wup'])
            S.dma('pool', aup[:, :], self.W['rw_a_up'][l].rearrange("d l c -> (d l) c"), writes=['aup'])
            S.dma('pool', gup[:], self.W['rw_g_up'][l], writes=['gup'])
            S.dma('sp', rmask[:], self.C['rmask'], writes=['rmask'])
            with ExitStack() as st0:
                tmpw = self.sb('tmpw', [128, T], F32, st0)
                S.dma('sp', tmpw[:], fT[768:896, :], writes=['tmpw'])
                S.op('act', lambda e: e.activation(out=lo[0:64, :], in_=tmpw[0:64, :], func=AF.Tanh), reads=['tmpw'], writes=['lo'])
                S.op('act', lambda e: e.copy(out=loa[:, :], in_=tmpw[64:128, :]), reads=['tmpw'], writes=['lo2'])
                S.dma('sp', tmpw[0:64, :], fT[896:960, :], reads=[], writes=['tmpw'])
                S.op('act', lambda e: e.activation(out=sgd[:], in_=tmpw[0:64, :], func=AF.Sigmoid), reads=['tmpw'], writes=['sgd'])
                S.barrier()
            for ct in range(2):
                with ExitStack() as st1:
                    sb1 = lambda n, shp, dt: self.sb(n, shp, dt, st1)
                    Ytok = sb1('Ytok', [128, 32, 64], F32)
                    khs = sb1('khs', [128, T], F32)
                    with ExitStack() as st2:
                        self._rwkv_scan(l, s, ct, st2, wup, aup, lo, loa, rmask, Ytok, khs)
                        S.barrier()
                    with ExitStack() as st3:
                        self._rwkv_final(l, s, ct, st3, gup, sgd, Ytok, khs)
                        S.barrier()

    def _rwkv_scan(self, l, s, ct, st, wup, aup, lo, loa, rmask, Ytok, khs):
        nc, S = self.nc, self.S
        PS = self.PS
        pp = self.pp
        fT = self.fT
        sb = lambda n, shp, dt: self.sb(n, shp, dt, st)
        NBD = 7
        bd = [[sb('bd%d_%d' % (i, j), [128, 8, 256 if j == 0 else 128], BF16) for j in range(NBD)] for i in range(2)]
        gC = [sb('gC%d' % i, [128, 8], F32) for i in range(2)]
        for i in range(2):
            for j in range(NBD):
                if j == 1:
                    continue
                S.op('pool', lambda e: e.memset(bd[i][j][:], 0.0), writes=[('bd', i, j)] + ([('bd', i, 1)] if j == 0 else []))
        f32t = {n: sb(n, [128, 512], F32) for n in ['r', 'k', 'v', 'kk', 'sq', 'sg', 'a', 'kh', 'b', 'cum', 'Bx', 'Cx', 'Dx', 'e1', 'e2', 'e3', 'e4']}
        tot = sb('tot', [128, 8], F32)
        Sbd = [sb('Sbd%d' % i, [128, 128], BF16) for i in range(2)]
        lanes = []
        for ln in range(3):
            lanes.append(dict(
                XP=[sb('XP%d_%d' % (ln, i), [128, 384], BF16) for i in range(2)],
                PT=[sb('PT%d_%d' % (ln, i), [128, 128], BF16) for i in range(2)],
                AkhT=sb('AkhT%d' % ln, [128, 128], BF16), ArkT=sb('ArkT%d' % ln, [128, 128], BF16),
                tok=sb('tok%d' % ln, [128, 5, 128], BF16),
                QpT=sb('QpT%d' % ln, [128, 128], BF16), PcT=sb('PcT%d' % ln, [128, 128], BF16),
                banks=(ln * 2, ln * 2 + 1, ln * 2 + 1), id=ln))
        PPREP = 6
        v3 = lambda ap: ap.rearrange("p (c t) -> p c t", t=64)
        state = {'cur': None, 'n': 0}

        def prep(di, g, bi):
            t0 = g * 512
            F = f32t
            rd = lambda n: [('f', n)]
            S.dma('sp', F['r'][:], fT[(0 + ct) * 128:(1 + ct) * 128, t0:t0 + 512], reads=[('fT', 0 + ct)], writes=rd('r'))
            yield
            S.dma('sp', F['k'][:], fT[(2 + ct) * 128:(3 + ct) * 128, t0:t0 + 512], reads=[('fT', 2 + ct)], writes=rd('k'))
            yield
            S.dma('sp', F['v'][:], fT[(4 + ct) * 128:(5 + ct) * 128, t0:t0 + 512], reads=[('fT', 4 + ct)], writes=rd('v'))
            yield
            S.op('dve', lambda e: e.tensor_scalar(out=F['kk'][:], in0=F['k'][:], scalar1=pp[:, 26 + ct:27 + ct], scalar2=None, op0=ALU.mult),
                 reads=rd('k') + ['pp'], writes=rd('kk'))
            yield
            S.op('pool', lambda e: e.tensor_tensor(out=F['sq'][:], in0=F['kk'][:], in1=F['kk'][:], op=ALU.mult), reads=rd('kk'), writes=rd('sq'))
            yield
            S.op('pe', lambda e: e.matmul(PS[PPREP][:, :], self.bonesF[:], F['sq'][:], start=True, stop=True),
                 reads=rd('sq') + ['bonesF'], writes=[('ps', PPREP)])
            yield
            S.op('act', lambda e: e.activation(out=F['sq'][:], in_=PS[PPREP][:, :], func=AF.Sqrt), reads=[('ps', PPREP)], writes=rd('sq'))
            yield
            S.op('dve', lambda e: e.tensor_scalar(out=F['sq'][:], in0=F['sq'][:], scalar1=1e-12, scalar2=None, op0=ALU.max), reads=rd('sq'), writes=rd('sq'))
            yield
            S.op('dve', lambda e: e.reciprocal(out=F['sq'][:], in_=F['sq'][:]), reads=rd('sq'), writes=rd('sq'))
            yield
            S.op('dve', lambda e: e.tensor_tensor(out=F['kk'][:], in0=F['kk'][:], in1=F['sq'][:], op=ALU.mult), reads=rd('kk') + rd('sq'), writes=rd('kk'))
            yield
            S.op('pe', lambda e: e.matmul(PS[PPREP + 1][:, :], wup[di * 32:(di + 1) * 32, ct * 128:(ct + 1) * 128], lo[di * 32:(di + 1) * 32, t0:t0 + 512],
                                          start=True, stop=True), reads=['wup', 'lo'], writes=[('ps', PPREP + 1)])
            yield
            S.op('act', lambda e: e.activation(out=F['sg'][:], in_=PS[PPREP + 1][:, :], func=AF.Sigmoid, bias=pp[:, 18 + di * 2 + ct:19 + di * 2 + ct]),
                 reads=[('ps', PPREP + 1), 'pp'], writes=rd('sg'))
            yield
            S.op('pe', lambda e: e.matmul(PS[PPREP][:, :], aup[di * 32:(di + 1) * 32, ct * 128:(ct + 1) * 128],
                                          loa[di * 32:(di + 1) * 32, t0:t0 + 512], start=True, stop=True),
                 reads=['aup', 'lo2'], writes=[('ps', PPREP)])
            yield
            S.op('act', lambda e: e.activation(out=F['a'][:], in_=PS[PPREP][:, :], func=AF.Sigmoid, bias=pp[:, 22 + di * 2 + ct:23 + di * 2 + ct]),
                 reads=[('ps', PPREP), 'pp'], writes=rd('a'))
            yield
            S.op('dve', lambda e: e.tensor_scalar(out=F['kh'][:], in0=F['a'][:], scalar1=pp[:, 28 + ct:29 + ct], scalar2=self.oka[:, ct:ct + 1],
                                                  op0=ALU.mult, op1=ALU.add), reads=rd('a') + ['pp', 'oka'], writes=rd('kh'))
            yield
            S.op('dve', lambda e: e.tensor_tensor(out=F['kh'][:], in0=F['kh'][:], in1=F['k'][:], op=ALU.mult), reads=rd('kh') + rd('k'), writes=rd('kh'))
            yield
            if di == 0:
                S.op('pool', lambda e: e.tensor_copy(out=khs[:, t0:t0 + 512], in_=F['kh'][:]), reads=rd('kh'), writes=[('khs', g)])
                yield
            else:
                S.op('pool', lambda e: e.tensor_tensor(out=khs[:, t0:t0 + 512], in0=khs[:, t0:t0 + 512], in1=F['kh'][:], op=ALU.add),
                     reads=rd('kh') + [('khs', g)], writes=[('khs', g)])
                yield
            S.op('pool', lambda e: e.tensor_tensor(out=F['b'][:], in0=F['kk'][:], in1=F['a'][:], op=ALU.mult), reads=rd('kk') + rd('a'), writes=rd('b'))
            yield
            S.op('dve', lambda e: e.tensor_tensor_scan(out=F['cum'][:], data0=rmask[:], data1=F['sg'][:], initial=0.0, op0=ALU.mult, op1=ALU.add),
                 reads=rd('sg') + ['rmask'], writes=rd('cum'))
            yield
            S.op('dve', lambda e: e.tensor_copy(out=tot[:], in_=v3(F['cum'][:])[:, :, 63]), reads=rd('cum'), writes=['tot'])
            yield
            S.op('dve', lambda e: e.tensor_tensor(out=F['Bx'][:], in0=F['cum'][:], in1=F['sg'][:], op=ALU.subtract), reads=rd('cum') + rd('sg'), writes=rd('Bx'))
            yield
            S.op('dve', lambda e: e.tensor_tensor(out=v3(F['Cx'][:]), in0=tot[:, :].unsqueeze(2).broadcast_to([128, 8, 64]), in1=v3(F['cum'][:]),
                                                  op=ALU.subtract), reads=rd('cum') + ['tot'], writes=rd('Cx'))
            yield
            if di == 0:
                ci, ce, en = 'cum', 'Bx', 'Cx'
            else:
                S.op('pool', lambda e: e.tensor_tensor(out=F['Dx'][:], in0=F['Cx'][:], in1=F['sg'][:], op=ALU.add), reads=rd('Cx') + rd('sg'), writes=rd('Dx'))
                yield
                ci, ce, en = 'Dx', 'Cx', 'Bx'
            S.op('act', lambda e: e.activation(out=F['e1'][:], in_=F[ci][:], func=AF.Exp, scale=C0), reads=rd(ci), writes=rd('e1'))
            yield
            S.op('act', lambda e: e.activation(out=F['e2'][:], in_=F[ce][:], func=AF.Exp, scale=C0), reads=rd(ce), writes=rd('e2'))
            yield
            S.op('act', lambda e: e.activation(out=F['e3'][:], in_=F[ci][:], func=AF.Exp, scale=-C0), reads=rd(ci), writes=rd('e3'))
            yield
            S.op('act', lambda e: e.activation(out=F['e4'][:], in_=F[en][:], func=AF.Exp, scale=C0), reads=rd(en), writes=rd('e4'))
            yield
            S.op('act', lambda e: e.activation(out=gC[bi][:], in_=tot[:], func=AF.Exp, scale=C0), reads=['tot'], writes=[('gC', bi)])
            yield
            prods = [(0, 'r', 'e1', 1.0), (1, 'kk', 'e2', 1.0), (2, 'kh', 'e3', 1.0), (3, 'b', 'e3', 1.0), (4, 'kh', 'e4', 1.0), (5, 'b', 'e4', -1.0)]
            cnt = 0
            for (j, an, bn, sc) in prods:
                for hh in range(2):
                    rows = slice(hh * 64, hh * 64 + 64)
                    if j == 0:
                        dst = bd[bi][0][rows, :, 128 + hh * 64:128 + hh * 64 + 64]
                    elif j == 1:
                        dst = bd[bi][0][rows, :, hh * 64:hh * 64 + 64]
                    else:
                        dst = bd[bi][j][rows, :, hh * 64:hh * 64 + 64]
                    eng = 'dve' if cnt % 3 != 2 else 'pool'
                    cnt += 1
                    if sc == 1.0:
                        S.op(eng, lambda e: e.tensor_tensor(out=dst, in0=v3(F[an][rows, :]), in1=v3(F[bn][rows, :]), op=ALU.mult),
                             reads=rd(an) + rd(bn), writes=[('bd', bi, j)])
                        yield
                    else:
                        S.op('dve', lambda e: e.scalar_tensor_tensor(out=dst, in0=v3(F[an][rows, :]), scalar=sc, in1=v3(F[bn][rows, :]),
                                                                   op0=ALU.mult, op1=ALU.mult), reads=rd(an) + rd(bn), writes=[('bd', bi, j)])
                        yield
            for hh in range(2):
                rows = slice(hh * 64, hh * 64 + 64)
                S.op('act', lambda e: e.copy(out=bd[bi][6][rows, :, hh * 64:hh * 64 + 64], in_=v3(F['v'][rows, :])), reads=rd('v'), writes=[('bd', bi, 6)])
                yield

        def chunk_gen(L, di, bi, cl, cabs):
            b0, b1, b2 = L['banks']
            lid = L['id']
            K = lambda n: ('L', lid, n)
            B = bd[bi]
            kkG, rG, QR = B[0][:, cl, 0:128], B[0][:, cl, 128:256], B[0][:, cl, :]
            kI, bI, kE, nbE, VT = B[2][:, cl, :], B[3][:, cl, :], B[4][:, cl, :], B[5][:, cl, :], B[6][:, cl, :]
            bdk = [('bd', bi, j) for j in range(7)]
            Mp, MpT, MpTi = ('mSL', 'mSU', 'mUI') if di == 0 else ('mSU', 'mSL', 'mLI')
            mm = lambda out, lhsT, rhs, rds, wr, start=True, stop=True: S.op('pe', lambda e: e.matmul(out, lhsT, rhs, start=start, stop=stop), reads=rds, writes=wr)
            XP, PT, tok = L['XP'], L['PT'], L['tok']
            pb2 = self.psb(b0)
            for j, src in enumerate((VT, kkG, kE, nbE)):
                sj = (6, 1, 4, 5)[j]
                S.op('pe', lambda e: e.transpose(pb2[:, j * 128:(j + 1) * 128], src, self.identB[:]), reads=[bdk[sj], 'identB'], writes=[('ps', b0)])
            S.op('act', lambda e: e.copy(out=tok[:, 0:4, :], in_=pb2[:, 0:512].rearrange("p (c t) -> p c t", c=4)), reads=[('ps', b0)], writes=[K('tok')])
            S.op('act', lambda e: e.copy(out=XP[0][:, 128:256], in_=pb2[:, 128:256]), reads=[('ps', b0)], writes=[K('Xb0')])
            mm(PS[b1][:, 0:256], kI, QR, [bdk[0], bdk[1], bdk[2]], [('ps', b1)])
            yield
            mm(PS[b0][:, 0:128], kkG, bI, [bdk[1], bdk[3]], [('ps', b0)])
            mm(PS[b0][:, 128:384], bI, QR, [bdk[0], bdk[1], bdk[3]], [('ps', b0)])
            stt = lambda out, in0, sc, in1, rds, wr: S.op('dve', lambda e: e.scalar_tensor_tensor(out=out, in0=in0, scalar=sc, in1=in1, op0=ALU.mult, op1=ALU.mult),
                                                          reads=rds, writes=wr)
            stt(L['AkhT'][:], PS[b1][:, 0:128], 1.0, self.masks[MpT][:], [('ps', b1), MpT], [K('AkhT')])
            stt(L['ArkT'][:], PS[b1][:, 128:256], 1.0, self.masks[MpTi][:], [('ps', b1), MpTi], [K('ArkT')])
            yield
            stt(XP[0][:, 256:384], PS[b0][:, 0:128], -1.0, self.masks[Mp][:], [('ps', b0), Mp], [K('P0')])
            stt(PT[0][:], PS[b0][:, 128:256], -1.0, self.masks[MpT][:], [('ps', b0), MpT], [K('PT0')])
            stt(tok[:, 4, :], PS[b0][:, 256:384], -1.0, self.masks[MpTi][:], [('ps', b0), MpTi], [K('nArbT')])
            yield
            Vbd, kkGbd, kEbd, nbEbd, nArbT = [tok[:, j, :] for j in range(5)]
            mm(PS[b1][:, 256:384], L['AkhT'][:], Vbd, [K('AkhT'), K('tok')], [('ps', b1)])
            S.op('act', lambda e: e.copy(out=XP[0][:, 0:128], in_=PS[b1][:, 256:384]), reads=[('ps', b1)], writes=[K('Xa0')])
            yield
            for k in range(6):
                c, n = k % 2, (k + 1) % 2
                ncol = 384 if k < 4 else 256
                rds = [K('PT%d' % c), K('Xa%d' % c), K('Xb%d' % c)] + ([K('P%d' % c)] if k < 4 else [])
                mm(PS[b0][:, 0:ncol], PT[c][:], XP[c][:, 0:ncol], rds, [('ps', b0)])
                if k < 5:
                    mm(PS[b1][:, 384:512], XP[c][:, 256:384], PT[c][:], [K('P%d' % c), K('PT%d' % c)], [('ps', b1)])
                S.op('dve', lambda e: e.tensor_tensor(out=XP[n][:, 0:256], in0=PS[b0][:, 0:256], in1=XP[c][:, 0:256], op=ALU.add),
                     reads=[('ps', b0), K('Xa%d' % c), K('Xb%d' % c)], writes=[K('Xa%d' % n), K('Xb%d' % n)])
                if k < 4:
                    S.op('act', lambda e: e.copy(out=XP[n][:, 256:384], in_=PS[b0][:, 256:384]), reads=[('ps', b0)], writes=[K('P%d' % n)])
                if k < 5:
                    S.op('act', lambda e: e.copy(out=PT[n][:], in_=PS[b1][:, 384:512]), reads=[('ps', b1)], writes=[K('PT%d' % n)])
                yield
            xf = [K('Xa0'), K('Xb0')]
            U0, M1 = XP[0][:, 0:128], XP[0][:, 128:256]
            mm(PS[b0][:, 0:256], M1, tok[:, 3:5, :].rearrange("p a b -> p (a b)"), xf + [K('tok'), K('nArbT')], [('ps', b0)])
            S.op('dve', lambda e: e.scalar_tensor_tensor(out=L['PcT'][:], in0=self.identB[:], scalar=gC[bi][:, cl:cl + 1], in1=PS[b0][:, 0:128],
                                                         op0=ALU.mult, op1=ALU.add), reads=[('ps', b0), 'identB', ('gC', bi)], writes=[K('PcT')])
            S.op('dve', lambda e: e.tensor_tensor(out=L['QpT'][:], in0=PS[b0][:, 128:256], in1=rG, op=ALU.add),
                 reads=[('ps', b0), bdk[0]], writes=[K('QpT')])
            yield
            first = state['cur'] is None
            pf2 = PS[b2]
            if not first:
                Sc = Sbd[state['cur']]
                sk = ('S', state['cur'])
                mm(pf2[:, 0:128], L['QpT'][:], Sc[:], [K('QpT'), sk], [('ps', b2)], start=True, stop=False)
            mm(pf2[:, 0:128], L['ArkT'][:], Vbd, [K('ArkT'), K('tok')], [('ps', b2)], start=first, stop=False)
            mm(pf2[:, 0:128], nArbT, U0, [K('nArbT')] + xf, [('ps', b2)], start=False, stop=True)
            if not first:
                mm(pf2[:, 128:256], L['PcT'][:], Sc[:], [K('PcT'), sk], [('ps', b2)], start=True, stop=False)
            mm(pf2[:, 128:256], kEbd, Vbd, [K('tok')], [('ps', b2)], start=first, stop=False)
            mm(pf2[:, 128:256], nbEbd, U0, [K('tok')] + xf, [('ps', b2)], start=False, stop=True)
            nxt = 0 if first else 1 - state['cur']
            S.op('act', lambda e: e.copy(out=Sbd[nxt][:], in_=pf2[:, 128:256]), reads=[('ps', b2)], writes=[('S', nxt)])
            state['cur'] = nxt
            for hh in range(2):
                rows = slice(hh * 64, hh * 64 + 64)
                src = pf2[rows, hh * 64:hh * 64 + 64]
                dst = Ytok[rows, cabs, :]
                if di == 0:
                    S.op('act', lambda e: e.copy(out=dst, in_=src), reads=[('ps', b2)], writes=[('Y', cabs)])
                else:
                    S.op('dve', lambda e: e.tensor_tensor(out=dst, in0=src, in1=dst, op=ALU.add), reads=[('ps', b2), ('Y', cabs)], writes=[('Y', cabs)])
            yield

        def run_chunks(di, items, extra=None):
            pending = list(items)
            act = []
            li = [0]
            while pending or act or extra is not None:
                if extra is not None:
                    try:
                        next(extra)
                    except StopIteration:
                        extra = None
                if pending and (len(act) == 0 or (len(act) < 3 and act[-1][1] >= 4)):
                    bi, cl, cabs = pending.pop(0)
                    L = lanes[li[0] % 3]
                    li[0] += 1
                    act.append([chunk_gen(L, di, bi, cl, cabs), 0])
                for it in list(act):
                    try:
                        next(it[0])
                        it[1] += 1
                    except StopIteration:
                        act.remove(it)

        for di in range(2):
            state['cur'] = None
            gorder = list(range(4)) if di == 0 else [3, 2, 1, 0]
            for _ in prep(di, gorder[0], 0):
                pass
            for gi, g in enumerate(gorder):
                bi = gi % 2
                extra = prep(di, gorder[gi + 1], (gi + 1) % 2) if gi + 1 < 4 else None
                corder = list(range(8)) if di == 0 else list(range(7, -1, -1))
                run_chunks(di, [(bi, cl, g * 8 + cl) for cl in corder], extra)

    def _rwkv_final(self, l, s, ct, st, gup, sgd, Ytok, khs):
        nc, S = self.nc, self.S
        PS = self.PS
        pp = self.pp
        fT = self.fT
        sb = lambda n, shp, dt: self.sb(n, shp, dt, st)
        ysq = sb('ysq', [128, 32, 64], F32)
        ynbd = sb('ynbd', [128, 32, 128], BF16)
        yfm = sb('yfm', [128, T], F32)
        rf = sb('rf', [128, T], F32)
        vf = sb('vf', [128, T], F32)
        pb = sb('pb', [128, T], BF16)
        s1 = sb('s1', [128, 32], F32)
        s2 = sb('s2', [128, 32], F32)
        t1 = [sb('t1_%d' % i, [128, 512], F32) for i in range(2)]
        S.dma('sp', rf[:], fT[(0 + ct) * 128:(1 + ct) * 128, :], writes=['rf'])
        S.dma('sp', vf[:], fT[(4 + ct) * 128:(5 + ct) * 128, :], writes=['vf'])
        S.op('pool', lambda e: e.memset(ynbd[:], 0.0), writes=['ynbd'])
        S.op('dve', lambda e: e.tensor_reduce(out=s1[:], in_=Ytok[:], axis=AX.X, op=ALU.add), reads=['Ytok'], writes=['s1'])
        S.op('dve', lambda e: e.tensor_tensor(out=ysq[:], in0=Ytok[:], in1=Ytok[:], op=ALU.mult), reads=['Ytok'], writes=['ysq'])
        S.op('dve', lambda e: e.tensor_reduce(out=s2[:], in_=ysq[:], axis=AX.X, op=ALU.add), reads=['ysq'], writes=['s2'])
        S.op('dve', lambda e: e.tensor_scalar(out=s1[:], in0=s1[:], scalar1=1.0 / 64, scalar2=None, op0=ALU.mult), reads=['s1'], writes=['s1'])
        S.op('dve', lambda e: e.tensor_tensor(out=ysq[:, :, 0], in0=s1[:], in1=s1[:], op=ALU.mult), reads=['s1', 'ysq'], writes=['ysq'])
        S.op('dve', lambda e: e.scalar_tensor_tensor(out=s2[:], in0=s2[:], scalar=1.0 / 64, in1=ysq[:, :, 0], op0=ALU.mult, op1=ALU.subtract),
             reads=['s2', 'ysq'], writes=['s2'])
        S.op('act', lambda e: e.activation(out=s2[:], in_=s2[:], func=AF.Sqrt, bias=GN_EPS, scale=1.0), reads=['s2'], writes=['s2'])
        S.op('dve', lambda e: e.reciprocal(out=s2[:], in_=s2[:]), reads=['s2'], writes=['s2'])
        S.op('dve', lambda e: e.tensor_tensor(out=ysq[:], in0=Ytok[:], in1=s1[:, :].unsqueeze(2).broadcast_to([128, 32, 64]), op=ALU.subtract),
             reads=['Ytok', 's1', 'ysq'], writes=['ysq'])
        for hh in range(2):
            rows = slice(hh * 64, hh * 64 + 64)
            S.op('dve', lambda e: e.tensor_tensor(out=ynbd[rows, :, hh * 64:hh * 64 + 64], in0=ysq[rows, :, :],
                                                  in1=s2[rows, :].unsqueeze(2).broadcast_to([64, 32, 64]), op=ALU.mult),
                 reads=['ysq', 's2', 'ynbd'], writes=['ynbd'])
        S.op('dve', lambda e: e.scalar_tensor_tensor(out=pb[:], in0=rf[:], scalar=pp[:, 30 + ct:31 + ct], in1=khs[:], op0=ALU.mult, op1=ALU.mult),
             reads=['rf', 'khs', 'pp'], writes=['pb'])
        for cb in range(4):
            pi = cb % 2
            psb = self.psb(pi)
            for j in range(8):
                c = cb * 8 + j
                S.op('pe', lambda e: e.transpose(psb[:, j * 128:(j + 1) * 128], ynbd[:, c, :], self.identB[:]), reads=['ynbd', 'identB'], writes=[('ps', pi)])
            for hh in range(2):
                rows = slice(hh * 64, hh * 64 + 64)
                src = psb[rows, :].rearrange("p (c t) -> p c t", c=8)[:, :, hh * 64:hh * 64 + 64]
                dst = yfm[rows, cb * 512:(cb + 1) * 512].rearrange("p (c t) -> p c t", c=8)
                S.op('act', lambda e: e.activation(out=dst, in_=src, func=AF.Identity, scale=pp[rows, 32 + ct:33 + ct], bias=pp[rows, 34 + ct:35 + ct]),
                     reads=[('ps', pi), 'pp'], writes=[('yfm', cb)])
            pbn = 2 + cb % 2
            pg = 4 + cb % 2
            S.op('pe', lambda e: e.matmul(PS[pbn][:, :], self.bonesB[:], pb[:, cb * 512:(cb + 1) * 512], start=True, stop=True),
                 reads=['pb', 'bonesB'], writes=[('ps', pbn)])
            S.op('pe', lambda e: e.matmul(PS[pg][:, :], gup[:, ct * 128:(ct + 1) * 128], sgd[:, cb * 512:(cb + 1) * 512], start=True, stop=True),
                 reads=['gup', 'sgd'], writes=[('ps', pg)])
            tt = t1[cb % 2]
            tk = ('t1', cb % 2)
            S.op('dve', lambda e: e.tensor_tensor(out=tt[:], in0=PS[pbn][:, :], in1=vf[:, cb * 512:(cb + 1) * 512], op=ALU.mult),
                 reads=[('ps', pbn), 'vf'], writes=[tk])
            S.op('dve', lambda e: e.tensor_tensor(out=tt[:], in0=tt[:], in1=yfm[:, cb * 512:(cb + 1) * 512], op=ALU.add),
                 reads=[tk, ('yfm', cb)], writes=[tk])
            S.op('dve', lambda e: e.tensor_tensor(out=self.yT[:, 2 + ct, cb * 512:(cb + 1) * 512], in0=PS[pg][:, :], in1=tt[:], op=ALU.mult),
                 reads=[('ps', pg), tk], writes=[('yT', 2 + ct)])

    def _ln(self, z, zk, g, b, st6, mv, rstd):
        S = self.S
        for nb in range(2):
            S.op('dve', lambda e: e.bn_stats(out=st6[:, nb, :], in_=z[:, nb * 512:(nb + 1) * 512]), reads=[zk], writes=['st6'])
        S.op('dve', lambda e: e.bn_aggr(out=mv[:], in_=st6[:].rearrange("p a b -> p (a b)")), reads=['st6'], writes=['mv'])
        S.op('act', lambda e: e.activation(out=rstd[:], in_=mv[:, 1:2], func=AF.Sqrt, bias=LN_EPS, scale=1.0), reads=['mv'], writes=['rstd'])
        S.op('dve', lambda e: e.reciprocal(out=rstd[:], in_=rstd[:]), reads=['rstd'], writes=['rstd'])
        S.op('dve', lambda e: e.tensor_scalar(out=z[:], in0=z[:], scalar1=mv[:, 0:1], scalar2=rstd[:, 0:1], op0=ALU.subtract, op1=ALU.mult),
             reads=[zk, 'mv', 'rstd'], writes=[zk])
        S.op('dve', lambda e: e.tensor_tensor(out=z[:], in0=z[:], in1=g[:], op=ALU.mult), reads=[zk, 'lng'], writes=[zk])
        S.op('pool', lambda e: e.tensor_tensor(out=z[:], in0=z[:], in1=b[:], op=ALU.add), reads=[zk, 'lnb'], writes=[zk])

    def stage_E(self, l, s, xsrc):
        nc, S = self.nc, self.S
        PS = self.PS
        rowp = self.W['rowp'][l]
        with ExitStack() as st:
            sb = lambda n, shp, dt: self.sb(n, shp, dt, st)
            wo = sb('wo', [128, 8, D], BF16)
            wr = sb('wr', [128, 8, 36], F32)
            lng = sb('lng', [128, D], F32)
            lnb = sb('lnb', [128, D], F32)
            rbias = sb('rbias', [128, 36], F32)
            xt = [sb('xt%d' % i, [128, D], F32) for i in range(2)]
            z = [sb('z%d' % i, [128, D], F32) for i in range(2)]
            x1b = [sb('x1b%d' % i, [128, D], BF16) for i in range(2)]
            x1T = sb('x1T', [128, 8, 128], F32)
            st6 = sb('st6', [128, 2, 6], F32)
            mv = sb('mv', [128, 2], F32)
            rstd = sb('rstd', [128, 1], F32)
            lg = sb('lg', [128, 36], F32)
            sm = {n: sb(n, [128, 1], F32) for n in ['gmax', 'gsum', 'gw', 'd21', 'e21', 'rden', 'd1', 'd2', 'p1', 'p2']}
            gsh = sb('gsh', [128, 4], F32)
            ge = sb('ge', [128, 4], F32)
            pen = sb('pen', [128, 4], F32)
            elm = sb('elm', [128, 32], F32)
            mx8 = sb('mx8', [128, 8], F32)
            oh1 = sb('oh1', [128, 32], F32)
            m2 = sb('m2', [128, 32], F32)
            oh2 = sb('oh2', [128, 32], F32)
            m2b = sb('m2b', [128, 32], BF16)
            pos = sb('pos', [128, 32], F32)
            sl = sb('sl', [128, 32], F32)
            tmp = sb('tmp32', [128, 32], F32)
            for k in range(8):
                S.dma('pool', wo[:, k, :], self.W['w_o'][l, k * 128:(k + 1) * 128, :], writes=[('wo', k)])
            S.dma('sp', wr[:, :, 0:4], self.W['router_group'][l].rearrange("(k p) g -> p k g", p=128), writes=['wr'])
            S.dma('sp', wr[:, :, 4:36], self.W['router_expert'][l].rearrange("(k p) g -> p k g", p=128), writes=['wr'])
            S.dma('sp', lng[:], rowp[:, 128:128 + D].broadcast_to([128, D]), writes=['lng'])
            S.dma('sp', lnb[:], rowp[:, 128 + D:128 + 2 * D].broadcast_to([128, D]), writes=['lnb'])
            S.dma('sp', rbias[:], rowp[:, 128 + 4 * D:128 + 4 * D + 36].broadcast_to([128, 36]), writes=['rbias'])
            wo_all = [('wo', k) for k in range(8)]
            yT_all = ['yT']
            for ti in range(16):
                gt = s * 16 + ti
                b = ti % 2
                xb, zb, zk = xt[b], z[b], ('z', b)
                S.dma('sp', xb[:], xsrc[gt * 128:(gt + 1) * 128, :], writes=[('xt', b)])
                for nb in range(2):
                    pi = b * 2 + nb
                    for k in range(8):
                        S.op('pe', lambda e: e.matmul(PS[pi][:, :], self.yT[:, k, ti * 128:(ti + 1) * 128], wo[:, k, nb * 512:(nb + 1) * 512],
                                                      start=(k == 0), stop=(k == 7)), reads=yT_all + wo_all, writes=[('ps', pi)])
                    S.op('dve', lambda e: e.scalar_tensor_tensor(out=zb[:, nb * 512:(nb + 1) * 512], in0=xb[:, nb * 512:(nb + 1) * 512], scalar=ALPHA,
                                                                 in1=PS[pi][:, :], op0=ALU.mult, op1=ALU.add),
                         reads=[('ps', pi), ('xt', b)], writes=[zk])
                self._ln(zb, zk, lng, lnb, st6, mv, rstd)
                S.dma('sp', self.x1d[gt * 128:(gt + 1) * 128, :], zb[:], reads=[zk], writes=[('x1d', gt)])
                S.op('act', lambda e: e.copy(out=x1b[b][:], in_=zb[:]), reads=[zk], writes=[('x1b', b)])
                for hf in range(2):
                    pi = 4 + hf
                    for c in range(4):
                        k = hf * 4 + c
                        S.op('pe', lambda e: e.transpose(PS[pi][:, c * 128:(c + 1) * 128], zb[:, k * 128:(k + 1) * 128], self.identF[:]),
                             reads=[zk, 'identF'], writes=[('ps', pi)])
                    src = PS[pi][:, :].rearrange("p (c t) -> p c t", c=4)
                    if hf == 0:
                        S.op('act', lambda e: e.copy(out=x1T[:, 0:4, :], in_=src), reads=[('ps', pi)], writes=['x1Ta'])
                    else:
                        S.op('dve', lambda e: e.tensor_copy(out=x1T[:, 4:8, :], in_=src), reads=[('ps', pi)], writes=['x1Tb'])
                for k in range(8):
                    S.op('pe', lambda e: e.matmul(PS[6][:, 0:36], x1T[:, k, :], wr[:, k, :], start=(k == 0), stop=(k == 7)),
                         reads=['x1Ta', 'x1Tb', 'wr'], writes=[('ps', 6)])
                D_ = lambda fn, rd, wr_: S.op('dve', fn, reads=rd, writes=wr_)
                D_(lambda e: e.tensor_tensor(out=lg[:], in0=PS[6][:, 0:36], in1=rbias[:], op=ALU.add), [('ps', 6), 'rbias'], ['lg'])
                D_(lambda e: e.tensor_reduce(out=sm['gmax'][:], in_=lg[:, 0:4], axis=AX.X, op=ALU.max), ['lg'], ['gmax'])
                D_(lambda e: e.tensor_scalar(out=gsh[:], in0=lg[:, 0:4], scalar1=sm['gmax'][:, 0:1], scalar2=None, op0=ALU.subtract), ['lg', 'gmax'], ['gsh'])
                S.op('act', lambda e: e.activation(out=ge[:], in_=gsh[:], func=AF.Exp, accum_out=sm['gsum'][:]), reads=['gsh'], writes=['ge', 'gsum'])
                D_(lambda e: e.reciprocal(out=sm['gw'][:], in_=sm['gsum'][:]), ['gsum'], ['gw'])
                D_(lambda e: e.tensor_scalar(out=pen[:], in0=gsh[:], scalar1=0.0, scalar2=None, op0=ALU.is_ge), ['gsh'], ['pen'])
                D_(lambda e: e.tensor_scalar(out=pen[:], in0=pen[:], scalar1=-1.0, scalar2=1e30, op0=ALU.add, op1=ALU.mult), ['pen'], ['pen'])
                D_(lambda e: e.tensor_tensor(out=elm[:].rearrange("p (g e) -> p g e", g=4), in0=lg[:, 4:36].rearrange("p (g e) -> p g e", g=4),
                                             in1=pen[:, :].unsqueeze(2).broadcast_to([128, 4, 8]), op=ALU.add), ['lg', 'pen'], ['elm'])
                D_(lambda e: e.max(out=mx8[:], in_=elm[:]), ['elm'], ['mx8'])
                D_(lambda e: e.tensor_scalar(out=oh1[:], in0=elm[:], scalar1=mx8[:, 0:1], scalar2=None, op0=ALU.is_ge), ['elm', 'mx8'], ['oh1'])
                D_(lambda e: e.tensor_scalar(out=m2[:], in0=elm[:], scalar1=mx8[:, 1:2], scalar2=None, op0=ALU.is_ge), ['elm', 'mx8'], ['m2'])
                D_(lambda e: e.tensor_tensor(out=oh2[:], in0=m2[:], in1=oh1[:], op=ALU.subtract), ['m2', 'oh1'], ['oh2'])
                D_(lambda e: e.tensor_tensor(out=sm['d21'][:], in0=mx8[:, 1:2], in1=mx8[:, 0:1], op=ALU.subtract), ['mx8'], ['d21'])
                S.op('act', lambda e: e.activation(out=sm['e21'][:], in_=sm['d21'][:], func=AF.Exp), reads=['d21'], writes=['e21'])
                D_(lambda e: e.tensor_scalar(out=sm['rden'][:], in0=sm['e21'][:], scalar1=1.0, scalar2=None, op0=ALU.add), ['e21'], ['rden'])
                D_(lambda e: e.reciprocal(out=sm['rden'][:], in_=sm['rden'][:]), ['rden'], ['rden'])
                D_(lambda e: e.tensor_tensor(out=self.rgate[:, gt, 0:1], in0=sm['gw'][:], in1=sm['rden'][:], op=ALU.mult), ['gw', 'rden'], [('rgate', gt)])
                D_(lambda e: e.tensor_tensor(out=self.rgate[:, gt, 1:2], in0=self.rgate[:, gt, 0:1], in1=sm['e21'][:], op=ALU.mult),
                   [('rgate', gt), 'e21'], [('rgate', gt)])
                D_(lambda e: e.tensor_copy(out=m2b[:], in_=m2[:]), ['m2'], ['m2b'])
                S.op('pe', lambda e: e.matmul(PS[7][:, 0:32], self.ustrict[:], m2b[:], start=True, stop=True), reads=['m2b', 'ustrict'], writes=[('ps', 7)])
                S.op('pe', lambda e: e.matmul(PS[7][:, 32:64], self.onesB[:], m2b[:], start=True, stop=True), reads=['m2b', 'onesB'], writes=[('ps', 7)])
                D_(lambda e: e.tensor_tensor(out=pos[:], in0=PS[7][:, 0:32], in1=self.runc[:], op=ALU.add), [('ps', 7), 'runc'], ['pos'])
                D_(lambda e: e.tensor_tensor(out=self.runc[:], in0=PS[7][:, 32:64], in1=self.runc[:], op=ALU.add), [('ps', 7), 'runc'], ['runc'])
                D_(lambda e: e.tensor_tensor(out=sl[:], in0=pos[:], in1=self.slotb[:], op=ALU.add), ['pos', 'slotb'], ['sl'])
                for j, oh in enumerate((oh1, oh2)):
                    dn, pn = ('d1', 'p1') if j == 0 else ('d2', 'p2')
                    ohk = 'oh1' if j == 0 else 'oh2'
                    D_(lambda e: e.tensor_tensor(out=tmp[:], in0=sl[:], in1=oh[:], op=ALU.mult), ['sl', ohk], ['tmp'])
                    D_(lambda e: e.tensor_reduce(out=sm[dn][:], in_=tmp[:], axis=AX.X, op=ALU.add), ['tmp'], [dn])
                    D_(lambda e: e.tensor_tensor(out=tmp[:], in0=pos[:], in1=oh[:], op=ALU.mult), ['pos', ohk], ['tmp'])
                    D_(lambda e: e.tensor_reduce(out=sm[pn][:], in_=tmp[:], axis=AX.X, op=ALU.add), ['tmp'], [pn])
                    D_(lambda e: e.tensor_scalar(out=sm[pn][:], in0=sm[pn][:], scalar1=float(CAP), scalar2=1e6, op0=ALU.is_ge, op1=ALU.mult), [pn], [pn])
                    D_(lambda e: e.tensor_tensor(out=sm[dn][:], in0=sm[dn][:], in1=sm[pn][:], op=ALU.add), [dn, pn], [dn])
                    D_(lambda e: e.tensor_scalar(out=sm[dn][:], in0=sm[dn][:], scalar1=float(NE * CAP), scalar2=None, op0=ALU.min), [dn], [dn])
                    D_(lambda e: e.tensor_copy(out=self.ridx[gt][j][:, :], in_=sm[dn][:]), [dn], [('ridx', gt, j)])
                    S.dma('pool', None, None, reads=[('ridx', gt, j), ('x1b', b)], writes=['xs'],
                          fn=lambda e: e.indirect_dma_start(out=self.xs[:, :], out_offset=bass.IndirectOffsetOnAxis(ap=self.ridx[gt][j][:, :], axis=0),
                                                            in_=x1b[b][:, :], in_offset=None))
            S.barrier()

    def stage_F(self, l):
        nc, S = self.nc, self.S
        PS = self.PS
        with ExitStack() as st:
            sb = lambda n, shp, dt: self.sb(n, shp, dt, st)
            wg = [sb('wg%d' % i, [128, 8, EH], BF16) for i in range(2)]
            wu = [sb('wu%d' % i, [128, 8, EH], BF16) for i in range(2)]
            wd = [sb('wd%d' % i, [128, 4, D], BF16) for i in range(2)]
            xsb = [sb('xsb%d' % i, [128, 3, D], BF16) for i in range(2)]
            xsT = sb('xsT', [128, 8, CAP], BF16)
            hT = sb('hT', [128, 4, CAP], BF16)
            sg = [sb('sg%d' % i, [128, CAP], F32) for i in range(2)]
            yo = [sb('yo%d' % i, [128, D], F32) for i in range(2)]

            def loads(e):
                b = e % 2
                S.dma('pool', wg[b][:], self.W['exp_gate'][l, e].rearrange("(k p) h -> p k h", p=128), writes=[('wg', b)])
                S.dma('pool', wu[b][:], self.W['exp_up'][l, e].rearrange("(k p) h -> p k h", p=128), writes=[('wu', b)])
                S.dma('pool', wd[b][:], self.W['exp_down'][l, e].rearrange("(k p) d -> p k d", p=128), writes=[('wd', b)])
                S.dma('sp', xsb[b][:], self.xs[e * CAP:(e + 1) * CAP, :].rearrange("(r p) d -> p r d", p=128), reads=['xs'], writes=[('xsb', b)])

            loads(0)
            yoi = 0
            for e in range(NE):
                b = e % 2
                if e + 1 < NE:
                    loads(e + 1)
                for r in range(3):
                    pi = r % 2
                    psb = self.psb(pi)
                    for k in range(8):
                        S.op('pe', lambda e_: e_.transpose(psb[:, k * 128:(k + 1) * 128], xsb[b][:, r, k * 128:(k + 1) * 128], self.identB[:]),
                             reads=[('xsb', b), 'identB'], writes=[('ps', pi)])
                    src = psb[:, :].rearrange("p (k t) -> p k t", k=8)
                    if r % 2 == 0:
                        S.op('act', lambda e_: e_.copy(out=xsT[:, :, r * 128:(r + 1) * 128], in_=src), reads=[('ps', pi)], writes=[('xsT', r)])
                    else:
                        S.op('dve', lambda e_: e_.tensor_copy(out=xsT[:, :, r * 128:(r + 1) * 128], in_=src), reads=[('ps', pi)], writes=[('xsT', r)])
                xsT_all = [('xsT', r) for r in range(3)]
                for hc in range(4):
                    pg, pu = 2 + (hc % 2) * 2, 3 + (hc % 2) * 2
                    for k in range(8):
                        S.op('pe', lambda e_: e_.matmul(PS[pg][:, 0:CAP], wg[b][:, k, hc * 128:(hc + 1) * 128], xsT[:, k, :], start=(k == 0), stop=(k == 7)),
                             reads=xsT_all + [('wg', b)], writes=[('ps', pg)])
                    for k in range(8):
                        S.op('pe', lambda e_: e_.matmul(PS[pu][:, 0:CAP], wu[b][:, k, hc * 128:(hc + 1) * 128], xsT[:, k, :], start=(k == 0), stop=(k == 7)),
                             reads=xsT_all + [('wu', b)], writes=[('ps', pu)])
                    sgb = sg[hc % 2]
                    S.op('act', lambda e_: e_.activation(out=sgb[:], in_=PS[pg][:, 0:CAP], func=AF.Silu), reads=[('ps', pg)], writes=[('sg', hc % 2)])
                    S.op('dve', lambda e_: e_.tensor_tensor(out=hT[:, hc, :], in0=PS[pu][:, 0:CAP], in1=sgb[:], op=ALU.mult),
                         reads=[('ps', pu), ('sg', hc % 2)], writes=[('hT', hc)])
                hT_all = [('hT', hc) for hc in range(4)]
                for r in range(3):
                    yb = yo[yoi % 2]
                    yk = ('yo', yoi % 2)
                    yoi += 1
                    for nb in range(2):
                        pi = 6 + nb
                        for hc in range(4):
                            S.op('pe', lambda e_: e_.matmul(PS[pi][:, :], hT[:, hc, r * 128:(r + 1) * 128], wd[b][:, hc, nb * 512:(nb + 1) * 512],
                                                            start=(hc == 0), stop=(hc == 3)), reads=hT_all + [('wd', b)], writes=[('ps', pi)])
                        if nb == 0:
                            S.op('act', lambda e_: e_.copy(out=yb[:, 0:512], in_=PS[pi][:, :]), reads=[('ps', pi)], writes=[yk])
                        else:
                            S.op('dve', lambda e_: e_.tensor_copy(out=yb[:, 512:1024], in_=PS[pi][:, :]), reads=[('ps', pi)], writes=[yk])
                    S.dma('sp', self.ys[e * CAP + r * 128:e * CAP + (r + 1) * 128, :], yb[:], reads=[yk], writes=['ys'])
            S.barrier()

    def stage_G(self, l, xdst):
        nc, S = self.nc, self.S
        rowp = self.W['rowp'][l]
        with ExitStack() as st:
            sb = lambda n, shp, dt: self.sb(n, shp, dt, st)
            lng = sb('lng2', [128, D], F32)
            lnb = sb('lnb2', [128, D], F32)
            y1 = [sb('y1_%d' % i, [128, D], F32) for i in range(2)]
            y2 = [sb('y2_%d' % i, [128, D], F32) for i in range(2)]
            x1t = [sb('x1t%d' % i, [128, D], F32) for i in range(2)]
            st6 = sb('st6g', [128, 2, 6], F32)
            mv = sb('mvg', [128, 2], F32)
            rstd = sb('rstdg', [128, 1], F32)
            S.dma('sp', lng[:], rowp[:, 128 + 2 * D:128 + 3 * D].broadcast_to([128, D]), writes=['lng'])
            S.dma('sp', lnb[:], rowp[:, 128 + 3 * D:128 + 4 * D].broadcast_to([128, D]), writes=['lnb'])
            for gt in range(32):
                b = gt % 2
                for j, yy in enumerate((y1[b], y2[b])):
                    yk = ('y', j, b)
                    S.op('pool', lambda e: e.memset(yy[:], 0.0), writes=[yk])
                    S.dma('pool', None, None, reads=['ys'], writes=[yk],
                          fn=lambda e: e.indirect_dma_start(out=yy[:, :], out_offset=None, in_=self.ys[:, :],
                                                            in_offset=bass.IndirectOffsetOnAxis(ap=self.ridx[gt][j][:, :], axis=0),
                                                            ))
                xb = x1t[b]
                xk = ('x1t', b)
                S.dma('sp', xb[:], self.x1d[gt * 128:(gt + 1) * 128, :], writes=[xk])
                S.op('dve', lambda e: e.tensor_scalar(out=y1[b][:], in0=y1[b][:], scalar1=self.rgate[:, gt, 0:1], scalar2=None, op0=ALU.mult),
                     reads=[('y', 0, b)], writes=[('y', 0, b)])
                S.op('dve', lambda e: e.scalar_tensor_tensor(out=y1[b][:], in0=y2[b][:], scalar=self.rgate[:, gt, 1:2], in1=y1[b][:],
                                                             op0=ALU.mult, op1=ALU.add), reads=[('y', 0, b), ('y', 1, b)], writes=[('y', 0, b)])
                S.op('dve', lambda e: e.scalar_tensor_tensor(out=xb[:], in0=xb[:], scalar=ALPHA, in1=y1[b][:], op0=ALU.mult, op1=ALU.add),
                     reads=[('y', 0, b), xk], writes=[xk])
                self._ln(xb, xk, lng, lnb, st6, mv, rstd)
                S.dma('sp', xdst[gt * 128:(gt + 1) * 128, :], xb[:], reads=[xk], writes=[('xdst', gt)])
            S.barrier()

    def run_layer(self, l, xsrc, xdst):
        nc, S = self.nc, self.S
        self.layer_setup(l)
        for s in range(NSEQ):
            with ExitStack() as st:
                sb = lambda n, shp, dt: self.sb(n, shp, dt, st)
                QT = sb('QT', [128, 4, T], BF16)
                KT = sb('KT', [128, 2, T], BF16)
                Vaug = sb('Vaug', [128, 16, 2, 128], BF16)
                if self.want('A'):
                    self.stage_A(l, s, xsrc, QT, KT, Vaug)
                if self.want('D'):
                    self.stage_D(QT, KT, Vaug)
            if self.want('C'):
                self.stage_C(l, s)
            if 'yT' in self.dbg_out and s == 0:
                S.dma('pool', self.dbg_out['yT'].rearrange("(k p) t -> p k t", p=128), self.yT[:], reads=[], writes=['dbg'])
                S.barrier()
            if self.want('E'):
                self.stage_E(l, s, xsrc)
        if self.want('F'):
            self.stage_F(l)
        if self.want('G'):
            self.stage_G(l, xdst)


def build(nl, dbg=None, stages=None):
    P = Prog(nl, dbg=dbg, stages=stages)
    for l in range(nl):
        xsrc = P.x_in if l == 0 else P.xres
        xdst = P.out if l == nl - 1 else P.xres
        P.run_layer(l, xsrc, xdst)
    return P.finish(), P


def make_in_maps(inputs, layers, xs_per_core):
    consts = _consts()
    nl = len(layers)
    shared = {}
    for k in ['w_in', 'pool_w', 'rw_w_up', 'rw_a_up', 'rw_g_up', 'w_o', 'router_group', 'router_expert',
              'exp_gate', 'exp_up', 'exp_down']:
        a = np.asarray(inputs[k])
        shared[k] = np.ascontiguousarray(a[layers[0]:layers[0] + nl]) if nl < a.shape[0] else np.ascontiguousarray(a)
    pps, rps = [], []
    for l in layers:
        p_, r_ = pack_small(inputs, l)
        pps.append(p_)
        rps.append(r_)
    shared['pp'] = np.stack(pps)
    shared['rowp'] = np.stack(rps)
    for k, v in consts.items():
        shared['c_' + k] = v
    maps = []
    for xc in xs_per_core:
        m = dict(shared)
        m['x'] = np.ascontiguousarray(xc)
        maps.append(m)
    return maps


FUSED = True
_NC_CACHE = {}


def _get_nc(nl):
    if nl not in _NC_CACHE:
        _NC_CACHE[nl] = build(nl)[0]
    return _NC_CACHE[nl]


def kernel(**inputs):
    x = np.asarray(inputs['x'], dtype=np.float32)
    xs = [np.ascontiguousarray(x[2 * c:2 * c + 2].reshape(NT, D)) for c in range(NCORES)]
    inp = {k: np.asarray(v) for k, v in inputs.items()}
    if FUSED:
        nc = _get_nc(DEPTH)
        maps = make_in_maps(inp, list(range(DEPTH)), xs)
        res = run_bass_kernel_spmd(nc, maps, core_ids=list(range(NCORES)))
        xs = [r['out'] for r in res.results]
    else:
        nc = _get_nc(1)
        for l in range(DEPTH):
            maps = make_in_maps(inp, [l], xs)
            res = run_bass_kernel_spmd(nc, maps, core_ids=list(range(NCORES)))
            xs = [np.ascontiguousarray(r['out']) for r in res.results]
    out = np.stack([np.asarray(xs[c]).reshape(2, T, D) for c in range(NCORES)]).reshape(NCORES * 2, T, D)
    return out.astype(np.float32)
```

```python
import math
from contextlib import ExitStack
import numpy as np
import concourse.bass as bass
import concourse.mybir as mybir
from concourse.bass_utils import run_bass_kernel_spmd

F32 = mybir.dt.float32
BF16 = mybir.dt.bfloat16
I32 = mybir.dt.int32
U32 = mybir.dt.uint32
AF = mybir.ActivationFunctionType
ALU = mybir.AluOpType
AX = mybir.AxisListType

NCORES = 8
D = 1024
T = 2048
NSEQ = 2
NT = NSEQ * T
DEPTH = 4
INW = 1984
NE = 32
EH = 512
CAP = 384
ALPHA = float((2 * DEPTH) ** 0.25)
LN_EPS = 1e-5
GN_EPS = 64e-5
QK_EPS = 1e-6
C0 = -math.exp(-0.5)
NPP = 36

SAME_ENGINE_SYNC = True
NDQ = 8


class Sch:
    def __init__(self, nc, ctx):
        self.nc = nc
        self.E = {'pe': nc.tensor, 'dve': nc.vector, 'act': nc.scalar,
                  'pool': nc.gpsimd, 'sp': nc.sync}
        self.csem = {}
        self.ccount = {}
        self.semobj = {}
        for e in ('pe', 'dve', 'act', 'pool'):
            self.csem[e] = ctx.enter_context(nc.semaphore('c_' + e))
            self.semobj[id(self.csem[e])] = self.csem[e]
            self.ccount[e] = 0
        self.dsem = {}
        self.dcount = {}
        for q in ('sp', 'pool', 'act'):
            self.dsem[q] = [ctx.enter_context(nc.semaphore('d_%s%d' % (q, i))) for i in range(NDQ)]
            for s in self.dsem[q]:
                self.semobj[id(s)] = s
            self.dcount[q] = 0
        self.seen = {e: {} for e in self.E}
        self.bufs = {}
        self.nwaits = 0
        self.nops = 0

    def _buf(self, k):
        b = self.bufs.get(k)
        if b is None:
            b = {'w': {}, 'r': {}}
            self.bufs[k] = b
        return b

    def _gather(self, reads, writes):
        deps = {}
        for k in reads:
            for s, v in self._buf(k)['w'].items():
                if deps.get(s, 0) < v:
                    deps[s] = v
        for k in writes:
            b = self._buf(k)
            for d in (b['w'], b['r']):
                for s, v in d.items():
                    if deps.get(s, 0) < v:
                        deps[s] = v
        return deps

    def _wait(self, eng, deps):
        own = id(self.csem[eng]) if eng in self.csem else None
        seen = self.seen[eng]
        for s, v in deps.items():
            if seen.get(s, 0) >= v:
                continue
            if s == own and (eng == 'pe' or not SAME_ENGINE_SYNC):
                continue
            self.E[eng].wait_ge(self.semobj[s], v)
            self.nwaits += 1
            seen[s] = v

    def _record(self, reads, writes, s, v):
        for k in reads:
            r = self._buf(k)['r']
            if r.get(s, 0) < v:
                r[s] = v
        for k in writes:
            b = self._buf(k)
            b['w'] = {s: v}
            b['r'] = {}

    @staticmethod
    def _norm(reads, writes):
        r2, w2 = [], []
        for k in reads:
            if isinstance(k, tuple) and k[0] == 'ps':
                w2.append(k[:2])
            else:
                r2.append(k)
        for k in writes:
            if isinstance(k, tuple) and k[0] == 'ps':
                w2.append(k[:2])
            else:
                w2.append(k)
        return r2, w2

    def op(self, eng, fn, reads=(), writes=()):
        reads, writes = self._norm(reads, writes)
        deps = self._gather(reads, writes)
        self._wait(eng, deps)
        ins = fn(self.E[eng])
        self.ccount[eng] += 1
        v = self.ccount[eng]
        sem = self.csem[eng]
        ins.then_inc(sem, 1)
        self._record(reads, writes, id(sem), v)
        self.nops += 1
        return ins

    def dma(self, q, out, in_, reads=(), writes=(), fn=None, **kw):
        n = self.dcount[q]
        sem = self.dsem[q][n % NDQ]
        s = id(sem)
        prev = 16 * (n // NDQ)
        deps = self._gather(reads, writes)
        if prev > 0 and deps.get(s, 0) < prev:
            deps[s] = prev
        self._wait(q, deps)
        if fn is not None:
            ins = fn(self.E[q])
        else:
            ins = self.E[q].dma_start(out=out, in_=in_, **kw)
        ins.then_inc(sem, 16)
        self.dcount[q] = n + 1
        self._record(reads, writes, s, prev + 16)
        self.nops += 1
        return ins

    def barrier(self):
        deps = {}
        for e, sem in self.csem.items():
            if self.ccount[e] > 0:
                deps[id(sem)] = self.ccount[e]
        for q, sems in self.dsem.items():
            n = self.dcount[q]
            for i, sem in enumerate(sems):
                cnt = (n - i + NDQ - 1) // NDQ
                if cnt > 0:
                    deps[id(sem)] = 16 * cnt
        for e in self.E:
            own = id(self.csem[e]) if e in self.csem else None
            d = {s: v for s, v in deps.items() if s != own}
            self._wait(e, d)
        self.bufs = {}


def _consts():
    c = {}
    c['identF'] = np.eye(128, dtype=np.float32)
    r = np.arange(128)
    same = (r[:, None] // 64) == (r[None, :] // 64)
    c['mSL'] = (same & (r[None, :] < r[:, None])).astype(np.float32)
    c['mSU'] = (same & (r[:, None] < r[None, :])).astype(np.float32)
    c['mLI'] = (same & (r[None, :] <= r[:, None])).astype(np.float32)
    c['mUI'] = (same & (r[:, None] <= r[None, :])).astype(np.float32)
    c['bones'] = same.astype(np.float32)
    c['ustrict'] = (r[:, None] < r[None, :]).astype(np.float32)
    c['ones'] = np.ones((128, 128), np.float32)
    t = np.arange(512)
    c['rmask'] = np.broadcast_to((t % 64 != 0).astype(np.float32), (128, 512)).copy()
    rows = T // 64
    row_id = np.repeat(np.arange(rows), 64).astype(np.float32)
    col_id = np.tile(np.arange(64), rows).astype(np.float32)
    half = 32
    inv_freq = (10000.0 ** (-np.arange(0, half, 2, dtype=np.float32) / half)).astype(np.float32)
    ang_r = row_id[:, None] * inv_freq
    ang_c = col_id[:, None] * inv_freq
    ang = np.concatenate([ang_r, ang_r, ang_c, ang_c], -1)
    c['cosT'] = np.cos(ang).astype(np.float32)
    sn = np.sin(ang).astype(np.float32)
    sgn = np.ones(64, np.float32)
    sgn[0:16] = -1.0
    sgn[32:48] = -1.0
    c['sinS'] = sn * sgn
    ic = np.zeros((2, 128, T), np.float32)
    tt = np.arange(T)
    for gi, w in enumerate((2, 4, 8, 16)):
        lo = np.clip(tt - w // 2, 0, T)
        hi = np.clip(tt + w - w // 2, 0, T)
        ic[gi // 2, (gi % 2) * 64:(gi % 2) * 64 + 64, :] = 1.0 / (hi - lo).astype(np.float32)
    c['icnt'] = ic
    c['slotb'] = np.broadcast_to((np.arange(NE) * CAP).astype(np.float32), (128, NE)).copy()
    return c


CONST_SHAPES = {k: v.shape for k, v in _consts().items()}

WNAMES = ['w_in', 'pool_w', 'rw_w_up', 'rw_a_up', 'rw_g_up', 'w_o', 'router_group', 'router_expert',
          'exp_gate', 'exp_up', 'exp_down', 'pp', 'rowp']
NROW = 2 * 64 + 4 * 1024 + 36


def pack_small(inp, l):
    pp = np.zeros((128, NPP), np.float32)
    mp = np.zeros(1024, np.float32); mp[:960] = inp['mu_prev'][l]
    mn = np.zeros(1024, np.float32); mn[:960] = inp['mu_next'][l]
    pp[:, 0:8] = mp.reshape(8, 128).T
    pp[:, 8:16] = mn.reshape(8, 128).T
    pp[:, 16:18] = inp['pool_scale'][l].reshape(2, 128).T
    pp[:, 18:22] = inp['rw_w0'][l].reshape(4, 128).T
    pp[:, 22:26] = inp['rw_a0'][l].reshape(4, 128).T
    pp[:, 26:28] = inp['rw_k_k'][l].reshape(2, 128).T
    pp[:, 28:30] = inp['rw_k_a'][l].reshape(2, 128).T
    pp[:, 30:32] = inp['rw_r_k'][l].reshape(2, 128).T
    pp[:, 32:34] = inp['rw_gn_g'][l].reshape(2, 128).T
    pp[:, 34:36] = inp['rw_gn_b'][l].reshape(2, 128).T
    rowp = np.concatenate([inp['q_norm'][l], inp['k_norm'][l], inp['ln1_g'][l], inp['ln1_b'][l],
                           inp['ln2_g'][l], inp['ln2_b'][l], inp['router_group_b'][l],
                           inp['router_expert_b'][l]]).astype(np.float32)
    return pp, rowp[None, :]


class Prog:
    def __init__(self, nl, dbg=None, stages=None):
        self.nl = nl
        self.dbg = dbg or {}
        self.stages = stages
        nc = self.nc = bass.Bass("TRN2", target_bir_lowering=False)
        self.ctx = ExitStack()
        ctx = self.ctx
        dt_in = lambda n, shp: nc.dram_tensor(n, list(shp), F32, kind="ExternalInput").ap()
        self.x_in = dt_in('x', [NT, D])
        self.W = {}
        self.W['w_in'] = dt_in('w_in', [nl, D, INW])
        self.W['pool_w'] = dt_in('pool_w', [nl, 4, 64, 64])
        self.W['rw_w_up'] = dt_in('rw_w_up', [nl, 2, 32, 256])
        self.W['rw_a_up'] = dt_in('rw_a_up', [nl, 2, 32, 256])
        self.W['rw_g_up'] = dt_in('rw_g_up', [nl, 64, 256])
        self.W['w_o'] = dt_in('w_o', [nl, D, D])
        self.W['router_group'] = dt_in('router_group', [nl, D, 4])
        self.W['router_expert'] = dt_in('router_expert', [nl, D, NE])
        self.W['exp_gate'] = dt_in('exp_gate', [nl, NE, D, EH])
        self.W['exp_up'] = dt_in('exp_up', [nl, NE, D, EH])
        self.W['exp_down'] = dt_in('exp_down', [nl, NE, EH, D])
        self.W['pp'] = dt_in('pp', [nl, 128, NPP])
        self.W['rowp'] = dt_in('rowp', [nl, 1, NROW])
        self.C = {k: dt_in('c_' + k, shp) for k, shp in CONST_SHAPES.items()}
        self.out = nc.dram_tensor('out', [NT, D], F32, kind="ExternalOutput").ap()
        self.dbg_out = {}
        for k, shp in self.dbg.items():
            self.dbg_out[k] = nc.dram_tensor('dbg_' + k, list(shp), F32, kind="ExternalOutput").ap()
        itn = lambda n, shp, dt: nc.dram_tensor(n, list(shp), dt, kind="Internal").ap()
        self.fT = itn('fT', [1024, T], F32)
        self.x1d = itn('x1d', [NT, D], F32)
        self.xres = itn('xres', [NT, D], F32)
        self.xs = itn('xs', [NE * CAP + 128, D], BF16)
        self.ys = itn('ys', [NE * CAP + 128, D], F32)
        self.S = Sch(nc, ctx)
        self._uid = 0

        def sb(n, shp, dt, _ctx=ctx):
            self._uid += 1
            return _ctx.enter_context(nc.sbuf_tensor('%s_u%d' % (n, self._uid), list(shp), dt))
        self.sb = sb
        self.PS = [ctx.enter_context(nc.psum_tensor('ps%d' % i, [128, 512], F32)) for i in range(8)]
        self.identF = sb('identF', [128, 128], F32)
        self.identB = sb('identB', [128, 128], BF16)
        self.masks = {k: sb(k, [128, 128], BF16) for k in ('mSL', 'mSU', 'mLI', 'mUI')}
        self.bonesF = sb('bonesF', [128, 128], F32)
        self.bonesB = sb('bonesB', [128, 128], BF16)
        self.ustrict = sb('ustrict', [128, 128], BF16)
        self.onesB = sb('onesB', [128, 128], BF16)
        self.slotb = sb('slotb', [128, NE], F32)
        self.pp = sb('pp', [128, NPP], F32)
        self.ppd = sb('ppd', [128, 8], F32)
        self.yT = sb('yT', [128, 8, T], BF16)
        self.ridx = [[sb('ridx%d_%d' % (g_, j_), [128, 1], I32) for j_ in range(2)] for g_ in range(32)]
        self.rgate = sb('rgate', [128, 32, 2], F32)
        self.runc = sb('runc', [128, NE], F32)
        S = self.S
        S.dma('sp', self.identF[:], self.C['identF'], writes=['identF'])
        S.dma('pool', self.identB[:], self.C['identF'], writes=['identB'])
        for k in self.masks:
            S.dma('pool', self.masks[k][:], self.C[k], writes=[k])
        S.dma('sp', self.bonesF[:], self.C['bones'], writes=['bonesF'])
        S.dma('pool', self.bonesB[:], self.C['bones'], writes=['bonesB'])
        S.dma('pool', self.ustrict[:], self.C['ustrict'], writes=['ustrict'])
        S.dma('pool', self.onesB[:], self.C['ones'], writes=['onesB'])
        S.dma('sp', self.slotb[:], self.C['slotb'], writes=['slotb'])
        with nc.sbuf_tensor('zinit', [128, D], F32) as zt:
            S.op('dve', lambda e: e.memset(zt[:], 0.0), writes=['zt'])
            S.dma('sp', self.ys[NE * CAP:NE * CAP + 128, :], zt[:], reads=['zt'], writes=['ys'])
            S.barrier()

    def ps(self, i):
        return self.PS[i]

    def psb(self, i):
        return self.PS[i][:].bitcast(BF16)

    def finish(self):
        S = self.S
        S.barrier()
        self.ctx.close()
        return self.nc

    def want(self, name):
        return self.stages is None or name in self.stages

    def layer_setup(self, l):
        nc, S = self.nc, self.S
        S.dma('sp', self.pp[:], self.W['pp'][l], writes=['pp'])
        self.csh = self.sb('csh%d' % l, [128, 8], F32)
        self.oka = self.sb('oka%d' % l, [128, 2], F32)
        S.op('dve', lambda e: e.tensor_tensor(out=self.csh[:], in0=self.pp[:, 0:8], in1=self.pp[:, 8:16], op=ALU.add),
             reads=['pp'], writes=['csh'])
        S.op('dve', lambda e: e.tensor_scalar(out=self.csh[:], in0=self.csh[:], scalar1=-1.0, scalar2=1.0,
                                              op0=ALU.mult, op1=ALU.add), reads=['csh'], writes=['csh'])
        S.op('dve', lambda e: e.memset(self.runc[:], 0.0), writes=['runc'])
        S.op('dve', lambda e: e.tensor_scalar(out=self.oka[:], in0=self.pp[:, 28:30], scalar1=-1.0, scalar2=1.0,
                                              op0=ALU.mult, op1=ALU.add), reads=['pp'], writes=['oka'])
        S.barrier()

    def stage_A(self, l, s, xsrc, QT, KT, Vaug):
        nc, S = self.nc, self.S
        PS = self.PS
        pp = self.pp
        rowp = self.W['rowp'][l]
        with ExitStack() as st:
            sb = lambda n, shp, dt: self.sb(n, shp, dt, st)
            xT = sb('xT', [128, 8, T], BF16)
            win = sb('win', [128, 8, INW], BF16)
            xt = [sb('xt%d' % i, [128, D], F32) for i in range(2)]
            rb = [sb('rb%d' % i, [128, T + 16], F32) for i in range(2)]
            tA = sb('tA', [128, T + 16], F32)
            tB = sb('tB', [128, T + 16], F32)
            dbf = sb('dbf', [128, T], BF16)
            icn = sb('icn', [128, T], F32)
            dm = icn
            pwbd = sb('pwbd', [128, 2, 128], BF16)
            gqk = sb('gqk', [128, 640], F32)
            cs = [sb('cs%d' % i, [128, 128], F32) for i in range(2)]
            qkv = [sb('qkv%d' % i, [128, 768], F32) for i in range(2)]
            wk1 = sb('wk1', [128, 640], F32)
            wk2 = sb('wk2', [128, 640], F32)
            ss = sb('ss', [128, 10], F32)
            qrb = [sb('qrb%d' % i, [128, 768], BF16) for i in range(2)]
            for k in range(8):
                S.dma('pool', win[:, k, :], self.W['w_in'][l, k * 128:(k + 1) * 128, :], writes=[('win', k)])
            S.op('dve', lambda e: e.memset(pwbd[:], 0.0), writes=['pwbd'])
            for gi in range(4):
                h = (gi % 2) * 64
                S.dma('pool', pwbd[h:h + 64, gi // 2, h:h + 64], self.W['pool_w'][l, gi], reads=[], writes=['pwbd'])
            for h in range(10):
                off = 0 if h < 8 else 64
                S.dma('sp', gqk[:, h * 64:(h + 1) * 64], rowp[:, off:off + 64].broadcast_to([128, 64]), writes=['gqk'])
            for i in range(2):
                S.op('dve', lambda e: e.memset(rb[i][:, 0:8], 0.0), writes=[('rb', i)])
                S.op('dve', lambda e: e.memset(rb[i][:, 8 + T:], 0.0), writes=[('rb', i)])
            S.op('pool', lambda e: e.memset(Vaug[:, :, :, 64:128], 1.0), writes=['Vaug'])
            for ti in range(16):
                xb = xt[ti % 2]
                S.dma('sp', xb[:], xsrc[s * T + ti * 128: s * T + (ti + 1) * 128, :], writes=[('xt', ti % 2)])
                for hf in range(2):
                    pi = (ti * 2 + hf) % 4
                    for c in range(4):
                        k = hf * 4 + c
                        S.op('pe', lambda e: e.transpose(PS[pi][:, c * 128:(c + 1) * 128], xb[:, k * 128:(k + 1) * 128],
                                                         self.identF[:]),
                             reads=[('xt', ti % 2), 'identF'], writes=[('ps', pi)])
                    src = PS[pi][:, :].rearrange("p (c t) -> p c t", c=4)
                    dst = xT[:, hf * 4:hf * 4 + 4, ti * 128:(ti + 1) * 128]
                    if hf == 0:
                        S.op('act', lambda e: e.copy(out=dst, in_=src), reads=[('ps', pi)], writes=[('xT', ti)])
                    else:
                        S.op('dve', lambda e: e.tensor_copy(out=dst, in_=src), reads=[('ps', pi)], writes=[('xT', ti)])
            xT_all = [('xT', ti) for ti in range(16)]
            win_all = [('win', k) for k in range(8)]
            for mt in range(10):
                msz = 128 if mt < 9 else 64
                r = rb[mt % 2]
                rk = ('rb', mt % 2)
                for tb in range(4):
                    pi = (mt * 4 + tb) % 4
                    for k in range(8):
                        S.op('pe', lambda e: e.matmul(PS[pi][0:msz, :], win[:, k, mt * 128:mt * 128 + msz],
                                                      xT[:, k, tb * 512:(tb + 1) * 512], start=(k == 0), stop=(k == 7)),
                             reads=xT_all + win_all, writes=[('ps', pi)])
                    dst = r[0:msz, 8 + tb * 512: 8 + (tb + 1) * 512]
                    if tb % 2 == 0:
                        S.op('act', lambda e: e.copy(out=dst, in_=PS[pi][0:msz, :]), reads=[('ps', pi)], writes=[rk])
                    else:
                        S.op('dve', lambda e: e.tensor_copy(out=dst, in_=PS[pi][0:msz, :]), reads=[('ps', pi)], writes=[rk])
                if mt >= 2:
                    m = mt - 2
                    f = tA if m % 2 == 0 else tB
                    fk = 'tA' if m % 2 == 0 else 'tB'
                    S.op('dve', lambda e: e.tensor_scalar(out=f[0:msz, 0:T], in0=r[0:msz, 8:8 + T], scalar1=self.csh[0:msz, m:m + 1],
                                                          scalar2=None, op0=ALU.mult), reads=[rk, 'csh'], writes=[fk])
                    S.op('dve', lambda e: e.scalar_tensor_tensor(out=f[0:msz, 0:T], in0=r[0:msz, 7:7 + T], scalar=pp[0:msz, m:m + 1],
                                                                 in1=f[0:msz, 0:T], op0=ALU.mult, op1=ALU.add),
                         reads=[rk, 'pp', fk], writes=[fk])
                    S.op('dve', lambda e: e.scalar_tensor_tensor(out=f[0:msz, 0:T], in0=r[0:msz, 9:9 + T], scalar=pp[0:msz, 8 + m:9 + m],
                                                                 in1=f[0:msz, 0:T], op0=ALU.mult, op1=ALU.add),
                         reads=[rk, 'pp', fk], writes=[fk])
                    S.dma('sp', self.fT[m * 128:m * 128 + msz, :], f[0:msz, 0:T], reads=[fk], writes=[('fT', m)])
                else:
                    ic = tB if mt == 0 else None
                    add = lambda o, a, b, rd, wr: S.op('dve', lambda e: e.tensor_tensor(out=o, in0=a, in1=b, op=ALU.add), reads=rd, writes=wr)
                    n2 = T + 15
                    add(tA[:, 0:n2], r[:, 0:n2], r[:, 1:n2 + 1], [rk], ['tA'])
                    n4 = T + 13
                    add(tB[:, 0:n4], tA[:, 0:n4], tA[:, 2:n4 + 2], ['tA'], ['tB'])
                    if mt == 0:
                        lo_src, lo_off, hi_src, hi_off, lok, hik = tA, 7, tB, 6, 'tA', 'tB'
                    else:
                        n8 = T + 9
                        add(tA[:, 0:n8], tB[:, 0:n8], tB[:, 4:n8 + 4], ['tB'], ['tA'])
                        n16 = T + 1
                        add(tB[:, 0:n16], tA[:, 0:n16], tA[:, 8:n16 + 8], ['tA'], ['tB'])
                        lo_src, lo_off, hi_src, hi_off, lok, hik = tA, 4, tB, 0, 'tA', 'tB'
                    S.dma('sp', icn[:], self.C['icnt'][mt], writes=['icn'])
                    S.op('dve', lambda e: e.tensor_tensor(out=dm[0:64, :], in0=lo_src[0:64, lo_off:lo_off + T], in1=icn[0:64, :], op=ALU.mult),
                         reads=[lok, 'icn'], writes=['icn'])
                    S.op('dve', lambda e: e.tensor_tensor(out=dm[64:128, :], in0=hi_src[64:128, hi_off:hi_off + T], in1=icn[64:128, :], op=ALU.mult),
                         reads=[hik, 'icn'], writes=['icn'])
                    S.op('dve', lambda e: e.tensor_tensor(out=dbf[:], in0=dm[:], in1=r[:, 8:8 + T], op=ALU.subtract),
                         reads=['icn', rk], writes=['dbf'])
                    for tb in range(4):
                        pi = 4 + tb % 2
                        S.op('pe', lambda e: e.matmul(PS[pi][:, :], pwbd[:, mt, :], dbf[:, tb * 512:(tb + 1) * 512], start=True, stop=True),
                             reads=['pwbd', 'dbf'], writes=[('ps', pi)])
                        S.op('act', lambda e: e.activation(out=self.yT[:, mt, tb * 512:(tb + 1) * 512], in_=PS[pi][:, :], func=AF.Identity,
                                                           scale=pp[:, 16 + mt:17 + mt]),
                             reads=[('ps', pi), 'pp'], writes=[('yT', mt)])
            for ti in range(16):
                b = ti % 2
                pq, pkv = 4 + b * 2, 5 + b * 2
                for k in range(8):
                    S.op('pe', lambda e: e.matmul(PS[pq][:, :], xT[:, k, ti * 128:(ti + 1) * 128], win[:, k, 1216:1728],
                                                  start=(k == 0), stop=(k == 7)), reads=xT_all + win_all, writes=[('ps', pq)])
                for k in range(8):
                    S.op('pe', lambda e: e.matmul(PS[pkv][:, 0:256], xT[:, k, ti * 128:(ti + 1) * 128], win[:, k, 1728:1984],
                                                  start=(k == 0), stop=(k == 7)), reads=xT_all + win_all, writes=[('ps', pkv)])
                qk = qkv[b]
                qkk = ('qkv', b)
                S.op('act', lambda e: e.copy(out=qk[:, 0:512], in_=PS[pq][:, :]), reads=[('ps', pq)], writes=[qkk])
                S.op('act', lambda e: e.copy(out=qk[:, 512:768], in_=PS[pkv][:, 0:256]), reads=[('ps', pkv)], writes=[qkk])
                c_t = cs[b]
                S.dma('sp', c_t[:, 0:64], self.C['cosT'][ti * 128:(ti + 1) * 128, :], writes=[('cs', b)])
                S.dma('sp', c_t[:, 64:128], self.C['sinS'][ti * 128:(ti + 1) * 128, :], writes=[('cs', b)])
                S.op('dve', lambda e: e.tensor_tensor(out=wk1[:], in0=qk[:, 0:640], in1=qk[:, 0:640], op=ALU.mult), reads=[qkk], writes=['wk1'])
                S.op('dve', lambda e: e.tensor_reduce(out=ss[:], in_=wk1[:].rearrange("p (h d) -> p h d", h=10), axis=AX.X, op=ALU.add),
                     reads=['wk1'], writes=['ss'])
                S.op('act', lambda e: e.activation(out=ss[:], in_=ss[:], func=AF.Sqrt, bias=QK_EPS, scale=1.0 / 64), reads=['ss'], writes=['ss'])
                S.op('dve', lambda e: e.reciprocal(out=ss[:], in_=ss[:]), reads=['ss'], writes=['ss'])
                S.op('dve', lambda e: e.tensor_tensor(out=wk1[:].rearrange("p (h d) -> p h d", h=10),
                                                      in0=qk[:, 0:640].rearrange("p (h d) -> p h d", h=10),
                                                      in1=ss[:, :].unsqueeze(2).broadcast_to([128, 10, 64]), op=ALU.mult),
                     reads=[qkk, 'ss'], writes=['wk1'])
                S.op('dve', lambda e: e.tensor_tensor(out=wk1[:], in0=wk1[:], in1=gqk[:], op=ALU.mult), reads=['wk1', 'gqk'], writes=['wk1'])
                v4 = lambda ap: ap.rearrange("p (h a q d) -> p h a q d", h=10, a=2, q=2)
                cos4 = c_t[:, 0:64].rearrange("p (a q d) -> p a q d", a=2, q=2)
                sin4 = c_t[:, 64:128].rearrange("p (a q d) -> p a q d", a=2, q=2)
                for a in range(2):
                    for q in range(2):
                        S.op('dve', lambda e: e.tensor_tensor(out=v4(wk2[:])[:, :, a, q, :], in0=v4(wk1[:])[:, :, a, 1 - q, :],
                                                              in1=sin4[:, a, q, :].unsqueeze(1).broadcast_to([128, 10, 16]), op=ALU.mult),
                             reads=['wk1', ('cs', b)], writes=['wk2'])
                S.op('dve', lambda e: e.tensor_tensor(out=wk1[:].rearrange("p (h d) -> p h d", h=10),
                                                      in0=wk1[:].rearrange("p (h d) -> p h d", h=10),
                                                      in1=c_t[:, 0:64].unsqueeze(1).broadcast_to([128, 10, 64]), op=ALU.mult),
                     reads=['wk1', ('cs', b)], writes=['wk1'])
                qr = qrb[b]
                qrk = ('qrb', b)
                S.op('dve', lambda e: e.tensor_tensor(out=qr[:, 0:512], in0=wk1[:, 0:512], in1=wk2[:, 0:512], op=ALU.add),
                     reads=['wk1', 'wk2'], writes=[qrk])
                for du in range(2):
                    S.op('dve', lambda e: e.tensor_tensor(out=qr[:, 512:768].rearrange("p (k u d) -> p k u d", k=2, u=2)[:, :, du, :],
                                                          in0=wk1[:, 512:640].rearrange("p (k d) -> p k d", k=2),
                                                          in1=wk2[:, 512:640].rearrange("p (k d) -> p k d", k=2), op=ALU.add),
                         reads=['wk1', 'wk2'], writes=[qrk])
                pt = 0 + b
                psb = self.psb(pt)
                for j in range(6):
                    S.op('pe', lambda e: e.transpose(psb[:, j * 128:(j + 1) * 128], qr[:, j * 128:(j + 1) * 128], self.identB[:]),
                         reads=[qrk, 'identB'], writes=[('ps', pt)])
                S.op('act', lambda e: e.copy(out=QT[:, :, ti * 128:(ti + 1) * 128], in_=psb[:, 0:512].rearrange("p (c t) -> p c t", c=4)),
                     reads=[('ps', pt)], writes=[('QT', ti)])
                S.op('act', lambda e: e.copy(out=KT[:, :, ti * 128:(ti + 1) * 128], in_=psb[:, 512:768].rearrange("p (c t) -> p c t", c=2)),
                     reads=[('ps', pt)], writes=[('KT', ti)])
                S.op('pool', lambda e: e.tensor_copy(out=Vaug[:, ti, :, 0:64], in_=qk[:, 640:768].rearrange("p (k d) -> p k d", k=2)),
                     reads=[qkk], writes=['Vaug'])
            S.barrier()

    def stage_D(self, QT, KT, Vaug):
        nc, S = self.nc, self.S
        PS = self.PS
        with ExitStack() as st:
            sb = lambda n, shp, dt: self.sb(n, shp, dt, st)
            NB = 4
            LOOK = 2
            pT = [sb('pT%d' % i, [128, 512], BF16) for i in range(NB)]
            rsum = [sb('rsum%d' % i, [64, 512], F32) for i in range(2)]
            allq = [('QT', ti) for ti in range(16)] + [('KT', ti) for ti in range(16)]
            seq = [(h, qb, kc) for h in range(8) for qb in range(4) for kc in range(16)]

            def emit_S(i):
                h, qb, kc = seq[i]
                kv, pr, hf = h // 4, h // 2, (h % 2) * 64
                pi = i % NB
                S.op('pe', lambda e: e.matmul(PS[pi][:, :], KT[hf:hf + 64, kv, kc * 128:(kc + 1) * 128],
                                              QT[hf:hf + 64, pr, qb * 512:(qb + 1) * 512], start=True, stop=True),
                     reads=allq, writes=[('ps', pi)])
                S.op('act', lambda e: e.activation(out=pT[pi][:], in_=PS[pi][:, :], func=AF.Exp, scale=0.125),
                     reads=[('ps', pi)], writes=[('pT', pi)])

            def emit_PV(i):
                h, qb, kc = seq[i]
                kv, pr, hf = h // 4, h // 2, (h % 2) * 64
                pi = i % NB
                blk = i // 16
                po = 6 + blk % 2
                S.op('pe', lambda e: e.matmul(PS[po][:, :], Vaug[:, kc, kv, :], pT[pi][:], start=(kc == 0), stop=(kc == 15)),
                     reads=[('pT', pi), 'Vaug'], writes=[('ps', po)])
                if kc == 15:
                    rs = rsum[blk % 2]
                    rsk = ('rsum', blk % 2)
                    S.op('act', lambda e: e.copy(out=rs[:], in_=PS[po][64:128, :]), reads=[('ps', po)], writes=[rsk])
                    S.op('dve', lambda e: e.reciprocal(out=rs[:], in_=rs[:]), reads=[rsk], writes=[rsk])
                    S.op('dve', lambda e: e.tensor_tensor(out=self.yT[hf:hf + 64, 4 + pr, qb * 512:(qb + 1) * 512], in0=PS[po][0:64, :],
                                                          in1=rs[:], op=ALU.mult), reads=[('ps', po), rsk], writes=[('yT', 4 + pr, hf)])

            for i in range(len(seq) + LOOK):
                if i < len(seq):
                    emit_S(i)
                if i >= LOOK:
                    emit_PV(i - LOOK)
            S.barrier()

    def stage_C(self, l, s):
        nc, S = self.nc, self.S
        PS = self.PS
        pp = self.pp
        fT = self.fT
        with ExitStack() as st:
            sb = lambda n, shp, dt: self.sb(n, shp, dt, st)
            wup = sb('wup', [64, 256], BF16)
            aup = sb('aup', [64, 256], BF16)
            loa = sb('loa', [64, T], BF16)
            gup = sb('gup', [64, 256], BF16)
            lo = sb('lo', [64, T], BF16)
            sgd = sb('sgd', [64, T], BF16)
            rmask = sb('rmask', [128, 512], F32)
            S.dma('pool', wup[:], self.W['rw_w_up'][l].rearrange("d l c -> (d l) c"), writes=['wup'])
            S.dma('pool', aup[:, :], self.W['rw_a_up'][l].rearrange("d l c -> (d l) c"), writes=['aup'])
            S.dma('pool', gup[:], self.W['rw_g_up'][l], writes=['gup'])
            S.dma('sp', rmask[:], self.C['rmask'], writes=['rmask'])
            with ExitStack() as st0:
                tmpw = self.sb('tmpw', [128, T], F32, st0)
                S.dma('sp', tmpw[:], fT[768:896, :], writes=['tmpw'])
                S.op('act', lambda e: e.activation(out=lo[0:64, :], in_=tmpw[0:64, :], func=AF.Tanh), reads=['tmpw'], writes=['lo'])
                S.op('act', lambda e: e.copy(out=loa[:, :], in_=tmpw[64:128, :]), reads=['tmpw'], writes=['lo2'])
                S.dma('sp', tmpw[0:64, :], fT[896:960, :], reads=[], writes=['tmpw'])
                S.op('act', lambda e: e.activation(out=sgd[:], in_=tmpw[0:64, :], func=AF.Sigmoid), reads=['tmpw'], writes=['sgd'])
                S.barrier()
            for ct in range(2):
                with ExitStack() as st1:
                    sb1 = lambda n, shp, dt: self.sb(n, shp, dt, st1)
                    Ytok = sb1('Ytok', [128, 32, 64], F32)
                    khs = sb1('khs', [128, T], F32)
                    with ExitStack() as st2:
                        self._rwkv_scan(l, s, ct, st2, wup, aup, lo, loa, rmask, Ytok, khs)
                        S.barrier()
                    with ExitStack() as st3:
                        self._rwkv_final(l, s, ct, st3, gup, sgd, Ytok, khs)
                        S.barrier()

    def _rwkv_scan(self, l, s, ct, st, wup, aup, lo, loa, rmask, Ytok, khs):
        nc, S = self.nc, self.S
        PS = self.PS
        pp = self.pp
        fT = self.fT
        sb = lambda n, shp, dt: self.sb(n, shp, dt, st)
        NBD = 7
        bd = [[sb('bd%d_%d' % (i, j), [128, 8, 256 if j == 0 else 128], BF16) for j in range(NBD)] for i in range(2)]
        gC = [sb('gC%d' % i, [128, 8], F32) for i in range(2)]
        for i in range(2):
            for j in range(NBD):
                if j == 1:
                    continue
                S.op('pool', lambda e: e.memset(bd[i][j][:], 0.0), writes=[('bd', i, j)] + ([('bd', i, 1)] if j == 0 else []))
        f32t = {n: sb(n, [128, 512], F32) for n in ['r', 'k', 'v', 'kk', 'sq', 'sg', 'a', 'kh', 'b', 'cum', 'Bx', 'Cx', 'Dx', 'e1', 'e2', 'e3', 'e4']}
        tot = sb('tot', [128, 8], F32)
        Sbd = [sb('Sbd%d' % i, [128, 128], BF16) for i in range(2)]
        lanes = []
        for ln in range(3):
            lanes.append(dict(
                XP=[sb('XP%d_%d' % (ln, i), [128, 384], BF16) for i in range(2)],
                PT=[sb('PT%d_%d' % (ln, i), [128, 128], BF16) for i in range(2)],
                AkhT=sb('AkhT%d' % ln, [128, 128], BF16), ArkT=sb('ArkT%d' % ln, [128, 128], BF16),
                tok=sb('tok%d' % ln, [128, 5, 128], BF16),
                QpT=sb('QpT%d' % ln, [128, 128], BF16), PcT=sb('PcT%d' % ln, [128, 128], BF16),
                banks=(ln * 2, ln * 2 + 1, ln * 2 + 1), id=ln))
        PPREP = 6
        v3 = lambda ap: ap.rearrange("p (c t) -> p c t", t=64)
        state = {'cur': None, 'n': 0}

        def prep(di, g, bi):
            t0 = g * 512
            F = f32t
            rd = lambda n: [('f', n)]
            S.dma('sp', F['r'][:], fT[(0 + ct) * 128:(1 + ct) * 128, t0:t0 + 512], reads=[('fT', 0 + ct)], writes=rd('r'))
            yield
            S.dma('sp', F['k'][:], fT[(2 + ct) * 128:(3 + ct) * 128, t0:t0 + 512], reads=[('fT', 2 + ct)], writes=rd('k'))
            yield
            S.dma('sp', F['v'][:], fT[(4 + ct) * 128:(5 + ct) * 128, t0:t0 + 512], reads=[('fT', 4 + ct)], writes=rd('v'))
            yield
            S.op('dve', lambda e: e.tensor_scalar(out=F['kk'][:], in0=F['k'][:], scalar1=pp[:, 26 + ct:27 + ct], scalar2=None, op0=ALU.mult),
                 reads=rd('k') + ['pp'], writes=rd('kk'))
            yield
            S.op('pool', lambda e: e.tensor_tensor(out=F['sq'][:], in0=F['kk'][:], in1=F['kk'][:], op=ALU.mult), reads=rd('kk'), writes=rd('sq'))
            yield
            S.op('pe', lambda e: e.matmul(PS[PPREP][:, :], self.bonesF[:], F['sq'][:], start=True, stop=True),
                 reads=rd('sq') + ['bonesF'], writes=[('ps', PPREP)])
            yield
            S.op('act', lambda e: e.activation(out=F['sq'][:], in_=PS[PPREP][:, :], func=AF.Sqrt), reads=[('ps', PPREP)], writes=rd('sq'))
            yield
            S.op('dve', lambda e: e.tensor_scalar(out=F['sq'][:], in0=F['sq'][:], scalar1=1e-12, scalar2=None, op0=ALU.max), reads=rd('sq'), writes=rd('sq'))
            yield
            S.op('dve', lambda e: e.reciprocal(out=F['sq'][:], in_=F['sq'][:]), reads=rd('sq'), writes=rd('sq'))
            yield
            S.op('dve', lambda e: e.tensor_tensor(out=F['kk'][:], in0=F['kk'][:], in1=F['sq'][:], op=ALU.mult), reads=rd('kk') + rd('sq'), writes=rd('kk'))
            yield
            S.op('pe', lambda e: e.matmul(PS[PPREP + 1][:, :], wup[di * 32:(di + 1) * 32, ct * 128:(ct + 1) * 128], lo[di * 32:(di + 1) * 32, t0:t0 + 512],
                                          start=True, stop=True), reads=['wup', 'lo'], writes=[('ps', PPREP + 1)])
            yield
            S.op('act', lambda e: e.activation(out=F['sg'][:], in_=PS[PPREP + 1][:, :], func=AF.Sigmoid, bias=pp[:, 18 + di * 2 + ct:19 + di * 2 + ct]),
                 reads=[('ps', PPREP + 1), 'pp'], writes=rd('sg'))
            yield
            S.op('pe', lambda e: e.matmul(PS[PPREP][:, :], aup[di * 32:(di + 1) * 32, ct * 128:(ct + 1) * 128],
                                          loa[di * 32:(di + 1) * 32, t0:t0 + 512], start=True, stop=True),
                 reads=['aup', 'lo2'], writes=[('ps', PPREP)])
            yield
            S.op('act', lambda e: e.activation(out=F['a'][:], in_=PS[PPREP][:, :], func=AF.Sigmoid, bias=pp[:, 22 + di * 2 + ct:23 + di * 2 + ct]),
                 reads=[('ps', PPREP), 'pp'], writes=rd('a'))
            yield
            S.op('dve', lambda e: e.tensor_scalar(out=F['kh'][:], in0=F['a'][:], scalar1=pp[:, 28 + ct:29 + ct], scalar2=self.oka[:, ct:ct + 1],
                                                  op0=ALU.mult, op1=ALU.add), reads=rd('a') + ['pp', 'oka'], writes=rd('kh'))
            yield
            S.op('dve', lambda e: e.tensor_tensor(out=F['kh'][:], in0=F['kh'][:], in1=F['k'][:], op=ALU.mult), reads=rd('kh') + rd('k'), writes=rd('kh'))
            yield
            if di == 0:
                S.op('pool', lambda e: e.tensor_copy(out=khs[:, t0:t0 + 512], in_=F['kh'][:]), reads=rd('kh'), writes=[('khs', g)])
                yield
            else:
                S.op('pool', lambda e: e.tensor_tensor(out=khs[:, t0:t0 + 512], in0=khs[:, t0:t0 + 512], in1=F['kh'][:], op=ALU.add),
                     reads=rd('kh') + [('khs', g)], writes=[('khs', g)])
                yield
            S.op('pool', lambda e: e.tensor_tensor(out=F['b'][:], in0=F['kk'][:], in1=F['a'][:], op=ALU.mult), reads=rd('kk') + rd('a'), writes=rd('b'))
            yield
            S.op('dve', lambda e: e.tensor_tensor_scan(out=F['cum'][:], data0=rmask[:], data1=F['sg'][:], initial=0.0, op0=ALU.mult, op1=ALU.add),
                 reads=rd('sg') + ['rmask'], writes=rd('cum'))
            yield
            S.op('dve', lambda e: e.tensor_copy(out=tot[:], in_=v3(F['cum'][:])[:, :, 63]), reads=rd('cum'), writes=['tot'])
            yield
            S.op('dve', lambda e: e.tensor_tensor(out=F['Bx'][:], in0=F['cum'][:], in1=F['sg'][:], op=ALU.subtract), reads=rd('cum') + rd('sg'), writes=rd('Bx'))
            yield
            S.op('dve', lambda e: e.tensor_tensor(out=v3(F['Cx'][:]), in0=tot[:, :].unsqueeze(2).broadcast_to([128, 8, 64]), in1=v3(F['cum'][:]),
                                                  op=ALU.subtract), reads=rd('cum') + ['tot'], writes=rd('Cx'))
            yield
            if di == 0:
                ci, ce, en = 'cum', 'Bx', 'Cx'
            else:
                S.op('pool', lambda e: e.tensor_tensor(out=F['Dx'][:], in0=F['Cx'][:], in1=F['sg'][:], op=ALU.add), reads=rd('Cx') + rd('sg'), writes=rd('Dx'))
                yield
                ci, ce, en = 'Dx', 'Cx', 'Bx'
            S.op('act', lambda e: e.activation(out=F['e1'][:], in_=F[ci][:], func=AF.Exp, scale=C0), reads=rd(ci), writes=rd('e1'))
            yield
            S.op('act', lambda e: e.activation(out=F['e2'][:], in_=F[ce][:], func=AF.Exp, scale=C0), reads=rd(ce), writes=rd('e2'))
            yield
            S.op('act', lambda e: e.activation(out=F['e3'][:], in_=F[ci][:], func=AF.Exp, scale=-C0), reads=rd(ci), writes=rd('e3'))
            yield
            S.op('act', lambda e: e.activation(out=F['e4'][:], in_=F[en][:], func=AF.Exp, scale=C0), reads=rd(en), writes=rd('e4'))
            yield
            S.op('act', lambda e: e.activation(out=gC[bi][:], in_=tot[:], func=AF.Exp, scale=C0), reads=['tot'], writes=[('gC', bi)])
            yield
            prods = [(0, 'r', 'e1', 1.0), (1, 'kk', 'e2', 1.0), (2, 'kh', 'e3', 1.0), (3, 'b', 'e3', 1.0), (4, 'kh', 'e4', 1.0), (5, 'b', 'e4', -1.0)]
            cnt = 0
            for (j, an, bn, sc) in prods:
                for hh in range(2):
                    rows = slice(hh * 64, hh * 64 + 64)
                    if j == 0:
                        dst = bd[bi][0][rows, :, 128 + hh * 64:128 + hh * 64 + 64]
                    elif j == 1:
                        dst = bd[bi][0][rows, :, hh * 64:hh * 64 + 64]
                    else:
                        dst = bd[bi][j][rows, :, hh * 64:hh * 64 + 64]
                    eng = 'dve' if cnt % 3 != 2 else 'pool'
                    cnt += 1
                    if sc == 1.0:
                        S.op(eng, lambda e: e.tensor_tensor(out=dst, in0=v3(F[an][rows, :]), in1=v3(F[bn][rows, :]), op=ALU.mult),
                             reads=rd(an) + rd(bn), writes=[('bd', bi, j)])
                        yield
                    else:
                        S.op('dve', lambda e: e.scalar_tensor_tensor(out=dst, in0=v3(F[an][rows, :]), scalar=sc, in1=v3(F[bn][rows, :]),
                                                                   op0=ALU.mult, op1=ALU.mult), reads=rd(an) + rd(bn), writes=[('bd', bi, j)])
                        yield
            for hh in range(2):
                rows = slice(hh * 64, hh * 64 + 64)
                S.op('act', lambda e: e.copy(out=bd[bi][6][rows, :, hh * 64:hh * 64 + 64], in_=v3(F['v'][rows, :])), reads=rd('v'), writes=[('bd', bi, 6)])
                yield

        def chunk_gen(L, di, bi, cl, cabs):
            b0, b1, b2 = L['banks']
            lid = L['id']
            K = lambda n: ('L', lid, n)
            B = bd[bi]
            kkG, rG, QR = B[0][:, cl, 0:128], B[0][:, cl, 128:256], B[0][:, cl, :]
            kI, bI, kE, nbE, VT = B[2][:, cl, :], B[3][:, cl, :], B[4][:, cl, :], B[5][:, cl, :], B[6][:, cl, :]
            bdk = [('bd', bi, j) for j in range(7)]
            Mp, MpT, MpTi = ('mSL', 'mSU', 'mUI') if di == 0 else ('mSU', 'mSL', 'mLI')
            mm = lambda out, lhsT, rhs, rds, wr, start=True, stop=True: S.op('pe', lambda e: e.matmul(out, lhsT, rhs, start=start, stop=stop), reads=rds, writes=wr)
            XP, PT, tok = L['XP'], L['PT'], L['tok']
            pb2 = self.psb(b0)
            for j, src in enumerate((VT, kkG, kE, nbE)):
                sj = (6, 1, 4, 5)[j]
                S.op('pe', lambda e: e.transpose(pb2[:, j * 128:(j + 1) * 128], src, self.identB[:]), reads=[bdk[sj], 'identB'], writes=[('ps', b0)])
            S.op('act', lambda e: e.copy(out=tok[:, 0:4, :], in_=pb2[:, 0:512].rearrange("p (c t) -> p c t", c=4)), reads=[('ps', b0)], writes=[K('tok')])
            S.op('act', lambda e: e.copy(out=XP[0][:, 128:256], in_=pb2[:, 128:256]), reads=[('ps', b0)], writes=[K('Xb0')])
            mm(PS[b1][:, 0:256], kI, QR, [bdk[0], bdk[1], bdk[2]], [('ps', b1)])
            yield
            mm(PS[b0][:, 0:128], kkG, bI, [bdk[1], bdk[3]], [('ps', b0)])
            mm(PS[b0][:, 128:384], bI, QR, [bdk[0], bdk[1], bdk[3]], [('ps', b0)])
            stt = lambda out, in0, sc, in1, rds, wr: S.op('dve', lambda e: e.scalar_tensor_tensor(out=out, in0=in0, scalar=sc, in1=in1, op0=ALU.mult, op1=ALU.mult),
                                                          reads=rds, writes=wr)
            stt(L['AkhT'][:], PS[b1][:, 0:128], 1.0, self.masks[MpT][:], [('ps', b1), MpT], [K('AkhT')])
            stt(L['ArkT'][:], PS[b1][:, 128:256], 1.0, self.masks[MpTi][:], [('ps', b1), MpTi], [K('ArkT')])
            yield
            stt(XP[0][:, 256:384], PS[b0][:, 0:128], -1.0, self.masks[Mp][:], [('ps', b0), Mp], [K('P0')])
            stt(PT[0][:], PS[b0][:, 128:256], -1.0, self.masks[MpT][:], [('ps', b0), MpT], [K('PT0')])
            stt(tok[:, 4, :], PS[b0][:, 256:384], -1.0, self.masks[MpTi][:], [('ps', b0), MpTi], [K('nArbT')])
            yield
            Vbd, kkGbd, kEbd, nbEbd, nArbT = [tok[:, j, :] for j in range(5)]
            mm(PS[b1][:, 256:384], L['AkhT'][:], Vbd, [K('AkhT'), K('tok')], [('ps', b1)])
            S.op('act', lambda e: e.copy(out=XP[0][:, 0:128], in_=PS[b1][:, 256:384]), reads=[('ps', b1)], writes=[K('Xa0')])
            yield
            for k in range(6):
                c, n = k % 2, (k + 1) % 2
                ncol = 384 if k < 4 else 256
                rds = [K('PT%d' % c), K('Xa%d' % c), K('Xb%d' % c)] + ([K('P%d' % c)] if k < 4 else [])
                mm(PS[b0][:, 0:ncol], PT[c][:], XP[c][:, 0:ncol], rds, [('ps', b0)])
                if k < 5:
                    mm(PS[b1][:, 384:512], XP[c][:, 256:384], PT[c][:], [K('P%d' % c), K('PT%d' % c)], [('ps', b1)])
                S.op('dve', lambda e: e.tensor_tensor(out=XP[n][:, 0:256], in0=PS[b0][:, 0:256], in1=XP[c][:, 0:256], op=ALU.add),
                     reads=[('ps', b0), K('Xa%d' % c), K('Xb%d' % c)], writes=[K('Xa%d' % n), K('Xb%d' % n)])
                if k < 4:
                    S.op('act', lambda e: e.copy(out=XP[n][:, 256:384], in_=PS[b0][:, 256:384]), reads=[('ps', b0)], writes=[K('P%d' % n)])
                if k < 5:
                    S.op('act', lambda e: e.copy(out=PT[n][:], in_=PS[b1][:, 384:512]), reads=[('ps', b1)], writes=[K('PT%d' % n)])
                yield
            xf = [K('Xa0'), K('Xb0')]
            U0, M1 = XP[0][:, 0:128], XP[0][:, 128:256]
            mm(PS[b0][:, 0:256], M1, tok[:, 3:5, :].rearrange("p a b -> p (a b)"), xf + [K('tok'), K('nArbT')], [('ps', b0)])
            S.op('dve', lambda e: e.scalar_tensor_tensor(out=L['PcT'][:], in0=self.identB[:], scalar=gC[bi][:, cl:cl + 1], in1=PS[b0][:, 0:128],
                                                         op0=ALU.mult, op1=ALU.add), reads=[('ps', b0), 'identB', ('gC', bi)], writes=[K('PcT')])
            S.op('dve', lambda e: e.tensor_tensor(out=L['QpT'][:], in0=PS[b0][:, 128:256], in1=rG, op=ALU.add),
                 reads=[('ps', b0), bdk[0]], writes=[K('QpT')])
            yield
            first = state['cur'] is None
            pf2 = PS[b2]
            if not first:
                Sc = Sbd[state['cur']]
                sk = ('S', state['cur'])
                mm(pf2[:, 0:128], L['QpT'][:], Sc[:], [K('QpT'), sk], [('ps', b2)], start=True, stop=False)
            mm(pf2[:, 0:128], L['ArkT'][:], Vbd, [K('ArkT'), K('tok')], [('ps', b2)], start=first, stop=False)
            mm(pf2[:, 0:128], nArbT, U0, [K('nArbT')] + xf, [('ps', b2)], start=False, stop=True)
            if not first:
                mm(pf2[:, 128:256], L['PcT'][:], Sc[:], [K('PcT'), sk], [('ps', b2)], start=True, stop=False)
            mm(pf2[:, 128:256], kEbd, Vbd, [K('tok')], [('ps', b2)], start=first, stop=False)
            mm(pf2[:, 128:256], nbEbd, U0, [K('tok')] + xf, [('ps', b2)], start=False, stop=True)
            nxt = 0 if first else 1 - state['cur']
            S.op('act', lambda e: e.copy(out=Sbd[nxt][:], in_=pf2[:, 128:256]), reads=[('ps', b2)], writes=[('S', nxt)])
            state['cur'] = nxt
            for hh in range(2):
                rows = slice(hh * 64, hh * 64 + 64)
                src = pf2[rows, hh * 64:hh * 64 + 64]
                dst = Ytok[rows, cabs, :]
                if di == 0:
                    S.op('act', lambda e: e.copy(out=dst, in_=src), reads=[('ps', b2)], writes=[('Y', cabs)])
                else:
                    S.op('dve', lambda e: e.tensor_tensor(out=dst, in0=src, in1=dst, op=ALU.add), reads=[('ps', b2), ('Y', cabs)], writes=[('Y', cabs)])
            yield

        def run_pass(di):
            state['cur'] = None
            gorder = list(range(4)) if di == 0 else [3, 2, 1, 0]
            corder = list(range(8)) if di == 0 else list(range(7, -1, -1))
            for _ in prep(di, gorder[0], 0):
                pass
            prep_done = {0}
            prep_gen, prep_for = prep(di, gorder[1], 1), 1
            want_prep = []
            pending = [(gi, gi % 2, cl, gorder[gi] * 8 + cl) for gi in range(4) for cl in corder]
            act = []
            fin = [0, 0, 0, 0]
            li = 0
            while pending or act or prep_gen is not None:
                if prep_gen is None and want_prep:
                    prep_for = want_prep.pop(0)
                    prep_gen = prep(di, gorder[prep_for], prep_for % 2)
                if prep_gen is not None:
                    for _ in range(2):
                        try:
                            next(prep_gen)
                        except StopIteration:
                            prep_done.add(prep_for)
                            prep_gen = None
                            break
                if pending and pending[0][0] in prep_done and (len(act) == 0 or (len(act) < 3 and act[-1][1] >= 4)):
                    gi, bi, cl, cabs = pending.pop(0)
                    L = lanes[li % 3]
                    li += 1
                    act.append([chunk_gen(L, di, bi, cl, cabs), 0, gi])
                for it in list(act):
                    try:
                        next(it[0])
                        it[1] += 1
                    except StopIteration:
                        act.remove(it)
                        fin[it[2]] += 1
                        if fin[it[2]] == 8 and it[2] + 2 < 4:
                            want_prep.append(it[2] + 2)

        for di in range(2):
            run_pass(di)

    def _rwkv_final(self, l, s, ct, st, gup, sgd, Ytok, khs):
        nc, S = self.nc, self.S
        PS = self.PS
        pp = self.pp
        fT = self.fT
        sb = lambda n, shp, dt: self.sb(n, shp, dt, st)
        ysq = sb('ysq', [128, 32, 64], F32)
        ynbd = sb('ynbd', [128, 32, 128], BF16)
        yfm = sb('yfm', [128, T], F32)
        rf = sb('rf', [128, T], F32)
        vf = sb('vf', [128, T], F32)
        pb = sb('pb', [128, T], BF16)
        s1 = sb('s1', [128, 32], F32)
        s2 = sb('s2', [128, 32], F32)
        t1 = [sb('t1_%d' % i, [128, 512], F32) for i in range(2)]
        S.dma('sp', rf[:], fT[(0 + ct) * 128:(1 + ct) * 128, :], writes=['rf'])
        S.dma('sp', vf[:], fT[(4 + ct) * 128:(5 + ct) * 128, :], writes=['vf'])
        S.op('pool', lambda e: e.memset(ynbd[:], 0.0), writes=['ynbd'])
        S.op('dve', lambda e: e.tensor_reduce(out=s1[:], in_=Ytok[:], axis=AX.X, op=ALU.add), reads=['Ytok'], writes=['s1'])
        S.op('dve', lambda e: e.tensor_tensor(out=ysq[:], in0=Ytok[:], in1=Ytok[:], op=ALU.mult), reads=['Ytok'], writes=['ysq'])
        S.op('dve', lambda e: e.tensor_reduce(out=s2[:], in_=ysq[:], axis=AX.X, op=ALU.add), reads=['ysq'], writes=['s2'])
        S.op('dve', lambda e: e.tensor_scalar(out=s1[:], in0=s1[:], scalar1=1.0 / 64, scalar2=None, op0=ALU.mult), reads=['s1'], writes=['s1'])
        S.op('dve', lambda e: e.tensor_tensor(out=ysq[:, :, 0], in0=s1[:], in1=s1[:], op=ALU.mult), reads=['s1', 'ysq'], writes=['ysq'])
        S.op('dve', lambda e: e.scalar_tensor_tensor(out=s2[:], in0=s2[:], scalar=1.0 / 64, in1=ysq[:, :, 0], op0=ALU.mult, op1=ALU.subtract),
             reads=['s2', 'ysq'], writes=['s2'])
        S.op('act', lambda e: e.activation(out=s2[:], in_=s2[:], func=AF.Sqrt, bias=GN_EPS, scale=1.0), reads=['s2'], writes=['s2'])
        S.op('dve', lambda e: e.reciprocal(out=s2[:], in_=s2[:]), reads=['s2'], writes=['s2'])
        S.op('dve', lambda e: e.tensor_tensor(out=ysq[:], in0=Ytok[:], in1=s1[:, :].unsqueeze(2).broadcast_to([128, 32, 64]), op=ALU.subtract),
             reads=['Ytok', 's1', 'ysq'], writes=['ysq'])
        for hh in range(2):
            rows = slice(hh * 64, hh * 64 + 64)
            S.op('dve', lambda e: e.tensor_tensor(out=ynbd[rows, :, hh * 64:hh * 64 + 64], in0=ysq[rows, :, :],
                                                  in1=s2[rows, :].unsqueeze(2).broadcast_to([64, 32, 64]), op=ALU.mult),
                 reads=['ysq', 's2', 'ynbd'], writes=['ynbd'])
        S.op('dve', lambda e: e.scalar_tensor_tensor(out=pb[:], in0=rf[:], scalar=pp[:, 30 + ct:31 + ct], in1=khs[:], op0=ALU.mult, op1=ALU.mult),
             reads=['rf', 'khs', 'pp'], writes=['pb'])
        for cb in range(4):
            pi = cb % 2
            psb = self.psb(pi)
            for j in range(8):
                c = cb * 8 + j
                S.op('pe', lambda e: e.transpose(psb[:, j * 128:(j + 1) * 128], ynbd[:, c, :], self.identB[:]), reads=['ynbd', 'identB'], writes=[('ps', pi)])
            for hh in range(2):
                rows = slice(hh * 64, hh * 64 + 64)
                src = psb[rows, :].rearrange("p (c t) -> p c t", c=8)[:, :, hh * 64:hh * 64 + 64]
                dst = yfm[rows, cb * 512:(cb + 1) * 512].rearrange("p (c t) -> p c t", c=8)
                S.op('act', lambda e: e.activation(out=dst, in_=src, func=AF.Identity, scale=pp[rows, 32 + ct:33 + ct], bias=pp[rows, 34 + ct:35 + ct]),
                     reads=[('ps', pi), 'pp'], writes=[('yfm', cb)])
            pbn = 2 + cb % 2
            pg = 4 + cb % 2
            S.op('pe', lambda e: e.matmul(PS[pbn][:, :], self.bonesB[:], pb[:, cb * 512:(cb + 1) * 512], start=True, stop=True),
                 reads=['pb', 'bonesB'], writes=[('ps', pbn)])
            S.op('pe', lambda e: e.matmul(PS[pg][:, :], gup[:, ct * 128:(ct + 1) * 128], sgd[:, cb * 512:(cb + 1) * 512], start=True, stop=True),
                 reads=['gup', 'sgd'], writes=[('ps', pg)])
            tt = t1[cb % 2]
            tk = ('t1', cb % 2)
            S.op('dve', lambda e: e.tensor_tensor(out=tt[:], in0=PS[pbn][:, :], in1=vf[:, cb * 512:(cb + 1) * 512], op=ALU.mult),
                 reads=[('ps', pbn), 'vf'], writes=[tk])
            S.op('dve', lambda e: e.tensor_tensor(out=tt[:], in0=tt[:], in1=yfm[:, cb * 512:(cb + 1) * 512], op=ALU.add),
                 reads=[tk, ('yfm', cb)], writes=[tk])
            S.op('dve', lambda e: e.tensor_tensor(out=self.yT[:, 2 + ct, cb * 512:(cb + 1) * 512], in0=PS[pg][:, :], in1=tt[:], op=ALU.mult),
                 reads=[('ps', pg), tk], writes=[('yT', 2 + ct)])

    def _ln(self, z, zk, g, b, st6, mv, rstd):
        S = self.S
        for nb in range(2):
            S.op('dve', lambda e: e.bn_stats(out=st6[:, nb, :], in_=z[:, nb * 512:(nb + 1) * 512]), reads=[zk], writes=['st6'])
        S.op('dve', lambda e: e.bn_aggr(out=mv[:], in_=st6[:].rearrange("p a b -> p (a b)")), reads=['st6'], writes=['mv'])
        S.op('act', lambda e: e.activation(out=rstd[:], in_=mv[:, 1:2], func=AF.Sqrt, bias=LN_EPS, scale=1.0), reads=['mv'], writes=['rstd'])
        S.op('dve', lambda e: e.reciprocal(out=rstd[:], in_=rstd[:]), reads=['rstd'], writes=['rstd'])
        S.op('dve', lambda e: e.tensor_scalar(out=z[:], in0=z[:], scalar1=mv[:, 0:1], scalar2=rstd[:, 0:1], op0=ALU.subtract, op1=ALU.mult),
             reads=[zk, 'mv', 'rstd'], writes=[zk])
        S.op('dve', lambda e: e.tensor_tensor(out=z[:], in0=z[:], in1=g[:], op=ALU.mult), reads=[zk, 'lng'], writes=[zk])
        S.op('pool', lambda e: e.tensor_tensor(out=z[:], in0=z[:], in1=b[:], op=ALU.add), reads=[zk, 'lnb'], writes=[zk])

    def stage_E(self, l, s, xsrc):
        nc, S = self.nc, self.S
        PS = self.PS
        rowp = self.W['rowp'][l]
        with ExitStack() as st:
            sb = lambda n, shp, dt: self.sb(n, shp, dt, st)
            wo = sb('wo', [128, 8, D], BF16)
            wr = sb('wr', [128, 8, 36], F32)
            lng = sb('lng', [128, D], F32)
            lnb = sb('lnb', [128, D], F32)
            rbias = sb('rbias', [128, 36], F32)
            xt = [sb('xt%d' % i, [128, D], F32) for i in range(2)]
            z = [sb('z%d' % i, [128, D], F32) for i in range(2)]
            x1b = [sb('x1b%d' % i, [128, D], BF16) for i in range(2)]
            x1T = sb('x1T', [128, 8, 128], F32)
            st6 = sb('st6', [128, 2, 6], F32)
            mv = sb('mv', [128, 2], F32)
            rstd = sb('rstd', [128, 1], F32)
            lg = sb('lg', [128, 36], F32)
            sm = {n: sb(n, [128, 1], F32) for n in ['gmax', 'gsum', 'gw', 'd21', 'e21', 'rden', 'd1', 'd2', 'p1', 'p2']}
            gsh = sb('gsh', [128, 4], F32)
            ge = sb('ge', [128, 4], F32)
            pen = sb('pen', [128, 4], F32)
            elm = sb('elm', [128, 32], F32)
            mx8 = sb('mx8', [128, 8], F32)
            oh1 = sb('oh1', [128, 32], F32)
            m2 = sb('m2', [128, 32], F32)
            oh2 = sb('oh2', [128, 32], F32)
            m2b = sb('m2b', [128, 32], BF16)
            pos = sb('pos', [128, 32], F32)
            sl = sb('sl', [128, 32], F32)
            tmp = sb('tmp32', [128, 32], F32)
            for k in range(8):
                S.dma('pool', wo[:, k, :], self.W['w_o'][l, k * 128:(k + 1) * 128, :], writes=[('wo', k)])
            S.dma('sp', wr[:, :, 0:4], self.W['router_group'][l].rearrange("(k p) g -> p k g", p=128), writes=['wr'])
            S.dma('sp', wr[:, :, 4:36], self.W['router_expert'][l].rearrange("(k p) g -> p k g", p=128), writes=['wr'])
            S.dma('sp', lng[:], rowp[:, 128:128 + D].broadcast_to([128, D]), writes=['lng'])
            S.dma('sp', lnb[:], rowp[:, 128 + D:128 + 2 * D].broadcast_to([128, D]), writes=['lnb'])
            S.dma('sp', rbias[:], rowp[:, 128 + 4 * D:128 + 4 * D + 36].broadcast_to([128, 36]), writes=['rbias'])
            wo_all = [('wo', k) for k in range(8)]
            yT_all = ['yT']
            for ti in range(16):
                gt = s * 16 + ti
                b = ti % 2
                xb, zb, zk = xt[b], z[b], ('z', b)
                S.dma('sp', xb[:], xsrc[gt * 128:(gt + 1) * 128, :], writes=[('xt', b)])
                for nb in range(2):
                    pi = b * 2 + nb
                    for k in range(8):
                        S.op('pe', lambda e: e.matmul(PS[pi][:, :], self.yT[:, k, ti * 128:(ti + 1) * 128], wo[:, k, nb * 512:(nb + 1) * 512],
                                                      start=(k == 0), stop=(k == 7)), reads=yT_all + wo_all, writes=[('ps', pi)])
                    S.op('dve', lambda e: e.scalar_tensor_tensor(out=zb[:, nb * 512:(nb + 1) * 512], in0=xb[:, nb * 512:(nb + 1) * 512], scalar=ALPHA,
                                                                 in1=PS[pi][:, :], op0=ALU.mult, op1=ALU.add),
                         reads=[('ps', pi), ('xt', b)], writes=[zk])
                self._ln(zb, zk, lng, lnb, st6, mv, rstd)
                S.dma('sp', self.x1d[gt * 128:(gt + 1) * 128, :], zb[:], reads=[zk], writes=[('x1d', gt)])
                S.op('act', lambda e: e.copy(out=x1b[b][:], in_=zb[:]), reads=[zk], writes=[('x1b', b)])
                for hf in range(2):
                    pi = 4 + hf
                    for c in range(4):
                        k = hf * 4 + c
                        S.op('pe', lambda e: e.transpose(PS[pi][:, c * 128:(c + 1) * 128], zb[:, k * 128:(k + 1) * 128], self.identF[:]),
                             reads=[zk, 'identF'], writes=[('ps', pi)])
                    src = PS[pi][:, :].rearrange("p (c t) -> p c t", c=4)
                    if hf == 0:
                        S.op('act', lambda e: e.copy(out=x1T[:, 0:4, :], in_=src), reads=[('ps', pi)], writes=['x1Ta'])
                    else:
                        S.op('dve', lambda e: e.tensor_copy(out=x1T[:, 4:8, :], in_=src), reads=[('ps', pi)], writes=['x1Tb'])
                for k in range(8):
                    S.op('pe', lambda e: e.matmul(PS[6][:, 0:36], x1T[:, k, :], wr[:, k, :], start=(k == 0), stop=(k == 7)),
                         reads=['x1Ta', 'x1Tb', 'wr'], writes=[('ps', 6)])
                D_ = lambda fn, rd, wr_: S.op('dve', fn, reads=rd, writes=wr_)
                D_(lambda e: e.tensor_tensor(out=lg[:], in0=PS[6][:, 0:36], in1=rbias[:], op=ALU.add), [('ps', 6), 'rbias'], ['lg'])
                D_(lambda e: e.tensor_reduce(out=sm['gmax'][:], in_=lg[:, 0:4], axis=AX.X, op=ALU.max), ['lg'], ['gmax'])
                D_(lambda e: e.tensor_scalar(out=gsh[:], in0=lg[:, 0:4], scalar1=sm['gmax'][:, 0:1], scalar2=None, op0=ALU.subtract), ['lg', 'gmax'], ['gsh'])
                S.op('act', lambda e: e.activation(out=ge[:], in_=gsh[:], func=AF.Exp, accum_out=sm['gsum'][:]), reads=['gsh'], writes=['ge', 'gsum'])
                D_(lambda e: e.reciprocal(out=sm['gw'][:], in_=sm['gsum'][:]), ['gsum'], ['gw'])
                D_(lambda e: e.tensor_scalar(out=pen[:], in0=gsh[:], scalar1=0.0, scalar2=None, op0=ALU.is_ge), ['gsh'], ['pen'])
                D_(lambda e: e.tensor_scalar(out=pen[:], in0=pen[:], scalar1=-1.0, scalar2=1e30, op0=ALU.add, op1=ALU.mult), ['pen'], ['pen'])
                D_(lambda e: e.tensor_tensor(out=elm[:].rearrange("p (g e) -> p g e", g=4), in0=lg[:, 4:36].rearrange("p (g e) -> p g e", g=4),
                                             in1=pen[:, :].unsqueeze(2).broadcast_to([128, 4, 8]), op=ALU.add), ['lg', 'pen'], ['elm'])
                D_(lambda e: e.max(out=mx8[:], in_=elm[:]), ['elm'], ['mx8'])
                D_(lambda e: e.tensor_scalar(out=oh1[:], in0=elm[:], scalar1=mx8[:, 0:1], scalar2=None, op0=ALU.is_ge), ['elm', 'mx8'], ['oh1'])
                D_(lambda e: e.tensor_scalar(out=m2[:], in0=elm[:], scalar1=mx8[:, 1:2], scalar2=None, op0=ALU.is_ge), ['elm', 'mx8'], ['m2'])
                D_(lambda e: e.tensor_tensor(out=oh2[:], in0=m2[:], in1=oh1[:], op=ALU.subtract), ['m2', 'oh1'], ['oh2'])
                D_(lambda e: e.tensor_tensor(out=sm['d21'][:], in0=mx8[:, 1:2], in1=mx8[:, 0:1], op=ALU.subtract), ['mx8'], ['d21'])
                S.op('act', lambda e: e.activation(out=sm['e21'][:], in_=sm['d21'][:], func=AF.Exp), reads=['d21'], writes=['e21'])
                D_(lambda e: e.tensor_scalar(out=sm['rden'][:], in0=sm['e21'][:], scalar1=1.0, scalar2=None, op0=ALU.add), ['e21'], ['rden'])
                D_(lambda e: e.reciprocal(out=sm['rden'][:], in_=sm['rden'][:]), ['rden'], ['rden'])
                D_(lambda e: e.tensor_tensor(out=self.rgate[:, gt, 0:1], in0=sm['gw'][:], in1=sm['rden'][:], op=ALU.mult), ['gw', 'rden'], [('rgate', gt)])
                D_(lambda e: e.tensor_tensor(out=self.rgate[:, gt, 1:2], in0=self.rgate[:, gt, 0:1], in1=sm['e21'][:], op=ALU.mult),
                   [('rgate', gt), 'e21'], [('rgate', gt)])
                D_(lambda e: e.tensor_copy(out=m2b[:], in_=m2[:]), ['m2'], ['m2b'])
                S.op('pe', lambda e: e.matmul(PS[7][:, 0:32], self.ustrict[:], m2b[:], start=True, stop=True), reads=['m2b', 'ustrict'], writes=[('ps', 7)])
                S.op('pe', lambda e: e.matmul(PS[7][:, 32:64], self.onesB[:], m2b[:], start=True, stop=True), reads=['m2b', 'onesB'], writes=[('ps', 7)])
                D_(lambda e: e.tensor_tensor(out=pos[:], in0=PS[7][:, 0:32], in1=self.runc[:], op=ALU.add), [('ps', 7), 'runc'], ['pos'])
                D_(lambda e: e.tensor_tensor(out=self.runc[:], in0=PS[7][:, 32:64], in1=self.runc[:], op=ALU.add), [('ps', 7), 'runc'], ['runc'])
                D_(lambda e: e.tensor_tensor(out=sl[:], in0=pos[:], in1=self.slotb[:], op=ALU.add), ['pos', 'slotb'], ['sl'])
                for j, oh in enumerate((oh1, oh2)):
                    dn, pn = ('d1', 'p1') if j == 0 else ('d2', 'p2')
                    ohk = 'oh1' if j == 0 else 'oh2'
                    D_(lambda e: e.tensor_tensor(out=tmp[:], in0=sl[:], in1=oh[:], op=ALU.mult), ['sl', ohk], ['tmp'])
                    D_(lambda e: e.tensor_reduce(out=sm[dn][:], in_=tmp[:], axis=AX.X, op=ALU.add), ['tmp'], [dn])
                    D_(lambda e: e.tensor_tensor(out=tmp[:], in0=pos[:], in1=oh[:], op=ALU.mult), ['pos', ohk], ['tmp'])
                    D_(lambda e: e.tensor_reduce(out=sm[pn][:], in_=tmp[:], axis=AX.X, op=ALU.add), ['tmp'], [pn])
                    D_(lambda e: e.tensor_scalar(out=sm[pn][:], in0=sm[pn][:], scalar1=float(CAP), scalar2=1e6, op0=ALU.is_ge, op1=ALU.mult), [pn], [pn])
                    D_(lambda e: e.tensor_tensor(out=sm[dn][:], in0=sm[dn][:], in1=sm[pn][:], op=ALU.add), [dn, pn], [dn])
                    D_(lambda e: e.tensor_scalar(out=sm[dn][:], in0=sm[dn][:], scalar1=float(NE * CAP), scalar2=None, op0=ALU.min), [dn], [dn])
                    D_(lambda e: e.tensor_copy(out=self.ridx[gt][j][:, :], in_=sm[dn][:]), [dn], [('ridx', gt, j)])
                    S.dma('pool', None, None, reads=[('ridx', gt, j), ('x1b', b)], writes=['xs'],
                          fn=lambda e: e.indirect_dma_start(out=self.xs[:, :], out_offset=bass.IndirectOffsetOnAxis(ap=self.ridx[gt][j][:, :], axis=0),
                                                            in_=x1b[b][:, :], in_offset=None))
            S.barrier()

    def stage_F(self, l):
        nc, S = self.nc, self.S
        PS = self.PS
        with ExitStack() as st:
            sb = lambda n, shp, dt: self.sb(n, shp, dt, st)
            wg = [sb('wg%d' % i, [128, 8, EH], BF16) for i in range(2)]
            wu = [sb('wu%d' % i, [128, 8, EH], BF16) for i in range(2)]
            wd = [sb('wd%d' % i, [128, 4, D], BF16) for i in range(2)]
            xsb = [sb('xsb%d' % i, [128, 3, D], BF16) for i in range(2)]
            xsT = sb('xsT', [128, 8, CAP], BF16)
            hT = sb('hT', [128, 4, CAP], BF16)
            sg = [sb('sg%d' % i, [128, CAP], F32) for i in range(2)]
            yo = [sb('yo%d' % i, [128, D], F32) for i in range(2)]

            def loads(e):
                b = e % 2
                S.dma('pool', wg[b][:], self.W['exp_gate'][l, e].rearrange("(k p) h -> p k h", p=128), writes=[('wg', b)])
                S.dma('pool', wu[b][:], self.W['exp_up'][l, e].rearrange("(k p) h -> p k h", p=128), writes=[('wu', b)])
                S.dma('pool', wd[b][:], self.W['exp_down'][l, e].rearrange("(k p) d -> p k d", p=128), writes=[('wd', b)])
                S.dma('sp', xsb[b][:], self.xs[e * CAP:(e + 1) * CAP, :].rearrange("(r p) d -> p r d", p=128), reads=['xs'], writes=[('xsb', b)])

            loads(0)
            yoi = 0
            for e in range(NE):
                b = e % 2
                if e + 1 < NE:
                    loads(e + 1)
                for r in range(3):
                    pi = r % 2
                    psb = self.psb(pi)
                    for k in range(8):
                        S.op('pe', lambda e_: e_.transpose(psb[:, k * 128:(k + 1) * 128], xsb[b][:, r, k * 128:(k + 1) * 128], self.identB[:]),
                             reads=[('xsb', b), 'identB'], writes=[('ps', pi)])
                    src = psb[:, :].rearrange("p (k t) -> p k t", k=8)
                    if r % 2 == 0:
                        S.op('act', lambda e_: e_.copy(out=xsT[:, :, r * 128:(r + 1) * 128], in_=src), reads=[('ps', pi)], writes=[('xsT', r)])
                    else:
                        S.op('dve', lambda e_: e_.tensor_copy(out=xsT[:, :, r * 128:(r + 1) * 128], in_=src), reads=[('ps', pi)], writes=[('xsT', r)])
                xsT_all = [('xsT', r) for r in range(3)]
                for hc in range(4):
                    pg, pu = 2 + (hc % 2) * 2, 3 + (hc % 2) * 2
                    for k in range(8):
                        S.op('pe', lambda e_: e_.matmul(PS[pg][:, 0:CAP], wg[b][:, k, hc * 128:(hc + 1) * 128], xsT[:, k, :], start=(k == 0), stop=(k == 7)),
                             reads=xsT_all + [('wg', b)], writes=[('ps', pg)])
                    for k in range(8):
                        S.op('pe', lambda e_: e_.matmul(PS[pu][:, 0:CAP], wu[b][:, k, hc * 128:(hc + 1) * 128], xsT[:, k, :], start=(k == 0), stop=(k == 7)),
                             reads=xsT_all + [('wu', b)], writes=[('ps', pu)])
                    sgb = sg[hc % 2]
                    S.op('act', lambda e_: e_.activation(out=sgb[:], in_=PS[pg][:, 0:CAP], func=AF.Silu), reads=[('ps', pg)], writes=[('sg', hc % 2)])
                    S.op('dve', lambda e_: e_.tensor_tensor(out=hT[:, hc, :], in0=PS[pu][:, 0:CAP], in1=sgb[:], op=ALU.mult),
                         reads=[('ps', pu), ('sg', hc % 2)], writes=[('hT', hc)])
                hT_all = [('hT', hc) for hc in range(4)]
                for r in range(3):
                    yb = yo[yoi % 2]
                    yk = ('yo', yoi % 2)
                    yoi += 1
                    for nb in range(2):
                        pi = 6 + nb
                        for hc in range(4):
                            S.op('pe', lambda e_: e_.matmul(PS[pi][:, :], hT[:, hc, r * 128:(r + 1) * 128], wd[b][:, hc, nb * 512:(nb + 1) * 512],
                                                            start=(hc == 0), stop=(hc == 3)), reads=hT_all + [('wd', b)], writes=[('ps', pi)])
                        if nb == 0:
                            S.op('act', lambda e_: e_.copy(out=yb[:, 0:512], in_=PS[pi][:, :]), reads=[('ps', pi)], writes=[yk])
                        else:
                            S.op('dve', lambda e_: e_.tensor_copy(out=yb[:, 512:1024], in_=PS[pi][:, :]), reads=[('ps', pi)], writes=[yk])
                    S.dma('sp', self.ys[e * CAP + r * 128:e * CAP + (r + 1) * 128, :], yb[:], reads=[yk], writes=['ys'])
            S.barrier()

    def stage_G(self, l, xdst):
        nc, S = self.nc, self.S
        rowp = self.W['rowp'][l]
        with ExitStack() as st:
            sb = lambda n, shp, dt: self.sb(n, shp, dt, st)
            lng = sb('lng2', [128, D], F32)
            lnb = sb('lnb2', [128, D], F32)
            y1 = [sb('y1_%d' % i, [128, D], F32) for i in range(2)]
            y2 = [sb('y2_%d' % i, [128, D], F32) for i in range(2)]
            x1t = [sb('x1t%d' % i, [128, D], F32) for i in range(2)]
            st6 = sb('st6g', [128, 2, 6], F32)
            mv = sb('mvg', [128, 2], F32)
            rstd = sb('rstdg', [128, 1], F32)
            S.dma('sp', lng[:], rowp[:, 128 + 2 * D:128 + 3 * D].broadcast_to([128, D]), writes=['lng'])
            S.dma('sp', lnb[:], rowp[:, 128 + 3 * D:128 + 4 * D].broadcast_to([128, D]), writes=['lnb'])
            for gt in range(32):
                b = gt % 2
                for j, yy in enumerate((y1[b], y2[b])):
                    yk = ('y', j, b)
                    S.op('pool', lambda e: e.memset(yy[:], 0.0), writes=[yk])
                    S.dma('pool', None, None, reads=['ys'], writes=[yk],
                          fn=lambda e: e.indirect_dma_start(out=yy[:, :], out_offset=None, in_=self.ys[:, :],
                                                            in_offset=bass.IndirectOffsetOnAxis(ap=self.ridx[gt][j][:, :], axis=0),
                                                            ))
                xb = x1t[b]
                xk = ('x1t', b)
                S.dma('sp', xb[:], self.x1d[gt * 128:(gt + 1) * 128, :], writes=[xk])
                S.op('dve', lambda e: e.tensor_scalar(out=y1[b][:], in0=y1[b][:], scalar1=self.rgate[:, gt, 0:1], scalar2=None, op0=ALU.mult),
                     reads=[('y', 0, b)], writes=[('y', 0, b)])
                S.op('dve', lambda e: e.scalar_tensor_tensor(out=y1[b][:], in0=y2[b][:], scalar=self.rgate[:, gt, 1:2], in1=y1[b][:],
                                                             op0=ALU.mult, op1=ALU.add), reads=[('y', 0, b), ('y', 1, b)], writes=[('y', 0, b)])
                S.op('dve', lambda e: e.scalar_tensor_tensor(out=xb[:], in0=xb[:], scalar=ALPHA, in1=y1[b][:], op0=ALU.mult, op1=ALU.add),
                     reads=[('y', 0, b), xk], writes=[xk])
                self._ln(xb, xk, lng, lnb, st6, mv, rstd)
                S.dma('sp', xdst[gt * 128:(gt + 1) * 128, :], xb[:], reads=[xk], writes=[('xdst', gt)])
            S.barrier()

    def run_layer(self, l, xsrc, xdst):
        nc, S = self.nc, self.S
        self.layer_setup(l)
        for s in range(NSEQ):
            with ExitStack() as st:
                sb = lambda n, shp, dt: self.sb(n, shp, dt, st)
                QT = sb('QT', [128, 4, T], BF16)
                KT = sb('KT', [128, 2, T], BF16)
                Vaug = sb('Vaug', [128, 16, 2, 128], BF16)
                if self.want('A'):
                    self.stage_A(l, s, xsrc, QT, KT, Vaug)
                if self.want('D'):
                    self.stage_D(QT, KT, Vaug)
            if self.want('C'):
                self.stage_C(l, s)
            if 'yT' in self.dbg_out and s == 0:
                S.dma('pool', self.dbg_out['yT'].rearrange("(k p) t -> p k t", p=128), self.yT[:], reads=[], writes=['dbg'])
                S.barrier()
            if self.want('E'):
                self.stage_E(l, s, xsrc)
        if self.want('F'):
            self.stage_F(l)
        if self.want('G'):
            self.stage_G(l, xdst)


def build(nl, dbg=None, stages=None):
    P = Prog(nl, dbg=dbg, stages=stages)
    for l in range(nl):
        xsrc = P.x_in if l == 0 else P.xres
        xdst = P.out if l == nl - 1 else P.xres
        P.run_layer(l, xsrc, xdst)
    return P.finish(), P


def make_in_maps(inputs, layers, xs_per_core):
    consts = _consts()
    nl = len(layers)
    shared = {}
    for k in ['w_in', 'pool_w', 'rw_w_up', 'rw_a_up', 'rw_g_up', 'w_o', 'router_group', 'router_expert',
              'exp_gate', 'exp_up', 'exp_down']:
        a = np.asarray(inputs[k])
        shared[k] = np.ascontiguousarray(a[layers[0]:layers[0] + nl]) if nl < a.shape[0] else np.ascontiguousarray(a)
    pps, rps = [], []
    for l in layers:
        p_, r_ = pack_small(inputs, l)
        pps.append(p_)
        rps.append(r_)
    shared['pp'] = np.stack(pps)
    shared['rowp'] = np.stack(rps)
    for k, v in consts.items():
        shared['c_' + k] = v
    maps = []
    for xc in xs_per_core:
        m = dict(shared)
        m['x'] = np.ascontiguousarray(xc)
        maps.append(m)
    return maps


FUSED = True
_NC_CACHE = {}


def _get_nc(nl):
    if nl not in _NC_CACHE:
        _NC_CACHE[nl] = build(nl)[0]
    return _NC_CACHE[nl]


def kernel(**inputs):
    x = np.asarray(inputs['x'], dtype=np.float32)
    xs = [np.ascontiguousarray(x[2 * c:2 * c + 2].reshape(NT, D)) for c in range(NCORES)]
    inp = {k: np.asarray(v) for k, v in inputs.items()}
    if FUSED:
        nc = _get_nc(DEPTH)
        maps = make_in_maps(inp, list(range(DEPTH)), xs)
        res = run_bass_kernel_spmd(nc, maps, core_ids=list(range(NCORES)))
        xs = [r['out'] for r in res.results]
    else:
        nc = _get_nc(1)
        for l in range(DEPTH):
            maps = make_in_maps(inp, [l], xs)
            res = run_bass_kernel_spmd(nc, maps, core_ids=list(range(NCORES)))
            xs = [np.ascontiguousarray(r['out']) for r in res.results]
    out = np.stack([np.asarray(xs[c]).reshape(2, T, D) for c in range(NCORES)]).reshape(NCORES * 2, T, D)
    return out.astype(np.float32)
```
